# Optimizing a Trainium2 kernel written in Bass

```python
import math
import jax
import jax.numpy as jnp
from jax import lax
import numpy as np

D_MODEL = 1024
BATCH = 16
SEQ = 2048
DEPTH = 1

GRID_W = 64
CTX_LEN = 256

DN_HEADS = 4
DN_DK = 128
DN_DV = 128
DN_QK = DN_HEADS * DN_DK
DN_WIDTH = DN_HEADS * DN_DV
DN_CONV_DIM = 2 * DN_QK + DN_WIDTH
SHORT_CONV = 7
CHUNK = 64

CF_CH = 512
CF_K = 31

D_MIX = DN_WIDTH + CF_CH
OFF_Z = DN_CONV_DIM
OFF_BA = OFF_Z + DN_WIDTH
OFF_CF = OFF_BA + 4 * DN_HEADS
D_IN = OFF_CF + 2 * CF_CH

N_EXPERTS = 64
TOP_K = 6
N_GROUPS = 8
TOPK_GROUPS = 4
D_EXPERT = 256
D_SHARED = 256
ROUTED_SCALE = 2.5
MOE_BLOCK = 128

EPS = 1e-6

kernel_name = 'hybrid_deltanet_conformer_moe_dit'


def rmsnorm(x, g):
    xf = x.astype(jnp.float32)
    y = xf * lax.rsqrt(jnp.mean(xf * xf, axis=-1, keepdims=True) + EPS)
    return (y * g.astype(jnp.float32)).astype(x.dtype)


def layernorm(x, g, b):
    xf = x.astype(jnp.float32)
    mu = jnp.mean(xf, axis=-1, keepdims=True)
    var = jnp.mean(jnp.square(xf - mu), axis=-1, keepdims=True)
    y = (xf - mu) * lax.rsqrt(var + EPS) * g.astype(jnp.float32) + b.astype(jnp.float32)
    return y.astype(x.dtype)


def l2norm(x):
    return x * lax.rsqrt(jnp.sum(x * x, axis=-1, keepdims=True) + EPS)


def modulate(h, shift, scale):
    return h * (1 + scale) + shift


def depthwise_conv_seq(x, w):
    k, ch = w.shape
    return lax.conv_general_dilated(x, w[:, None, :].astype(x.dtype), (1,), [(k // 2, k // 2)],
                                    dimension_numbers=('NWC', 'WIO', 'NWC'), feature_group_count=ch)


def depthwise_conv_grid_columns(x, w):
    bsz, n, ch = x.shape
    rows = n // GRID_W
    k = w.shape[0]
    y = lax.conv_general_dilated(x.reshape(bsz, rows, GRID_W, ch), w[:, None, None, :].astype(x.dtype),
                                 (1, 1), [(k // 2, k // 2), (0, 0)],
                                 dimension_numbers=('NHWC', 'HWIO', 'NHWC'), feature_group_count=ch)
    return y.reshape(bsz, n, ch)


def split_mixer_inputs(u):
    return u[..., :OFF_Z], u[..., OFF_Z:OFF_BA], u[..., OFF_BA:OFF_CF], u[..., OFF_CF:]


def delta_inputs(qkv, ba, conv_w, a_log, dt_bias):
    bsz, n, _ = qkv.shape
    qkv = jax.nn.silu(depthwise_conv_seq(qkv, conv_w)).astype(jnp.float32)
    q = l2norm(qkv[..., :DN_QK].reshape(bsz, n, DN_HEADS, DN_DK)) * (DN_DK ** -0.5)
    k = l2norm(qkv[..., DN_QK:2 * DN_QK].reshape(bsz, n, DN_HEADS, DN_DK))
    v = qkv[..., 2 * DN_QK:].reshape(bsz, n, DN_HEADS, DN_DV)
    ba = ba.astype(jnp.float32).reshape(bsz, n, 2, 2, DN_HEADS)
    beta = jax.nn.sigmoid(ba[:, :, :, 0])
    g = -jnp.exp(a_log.astype(jnp.float32)) * jax.nn.softplus(ba[:, :, :, 1] + dt_bias.astype(jnp.float32))
    return q, k, v, beta, g


def gated_delta_chunked(q, k, v, g, beta, s0):
    bsz, n, heads, dk = q.shape
    dv = v.shape[-1]
    nc = n // CHUNK

    def blocks(t):
        t = t.reshape(bsz, nc, CHUNK, heads, *t.shape[3:])
        return jnp.moveaxis(t, (1, 3), (0, 2))

    qc, kc, vc, bc = blocks(q), blocks(k), blocks(v), blocks(beta)
    gc = jnp.cumsum(blocks(g), axis=-1)
    tri_incl = jnp.tril(jnp.ones((CHUNK, CHUNK), dtype=bool))
    tri_strict = jnp.tril(jnp.ones((CHUNK, CHUNK), dtype=bool), -1)
    decay = jnp.exp(jnp.where(tri_incl, gc[..., :, None] - gc[..., None, :], -jnp.inf))
    kb = kc * bc[..., None]
    lmat = jnp.where(tri_strict, jnp.einsum('nbhik,nbhjk->nbhij', kb, kc) * decay, 0.0)
    eye = jnp.eye(CHUNK, dtype=lmat.dtype)
    tinv = lax.linalg.triangular_solve(eye + lmat, jnp.broadcast_to(eye, lmat.shape),
                                       left_side=True, lower=True, unit_diagonal=True)
    u = jnp.einsum('nbhij,nbhjv->nbhiv', tinv, vc * bc[..., None])
    w = jnp.einsum('nbhij,nbhjk->nbhik', tinv, kb * jnp.exp(gc)[..., None])
    intra = jnp.einsum('nbhik,nbhjk->nbhij', qc, kc) * decay
    qd = qc * jnp.exp(gc)[..., None]
    kd = kc * jnp.exp(gc[..., -1:] - gc)[..., None]
    glast = jnp.exp(gc[..., -1])

    def step(s, inp):
        u_i, w_i, a_i, qd_i, kd_i, gl_i = inp
        v_new = u_i - jnp.einsum('bhck,bhkv->bhcv', w_i, s)
        o = jnp.einsum('bhck,bhkv->bhcv', qd_i, s) + jnp.einsum('bhij,bhjv->bhiv', a_i, v_new)
        s = s * gl_i[..., None, None] + jnp.einsum('bhck,bhcv->bhkv', kd_i, v_new)
        return s, o

    s_fin, o = lax.scan(step, s0, (u, w, intra, qd, kd, glast))
    o = jnp.moveaxis(o, (0, 2), (1, 3)).reshape(bsz, n, heads, dv)
    return o, s_fin


def bidir_gated_delta(q, k, v, beta, g, s0_fwd, s0_bwd):
    o_f, s_f = gated_delta_chunked(q, k, v, g[:, :, 0], beta[:, :, 0], s0_fwd)
    fl = lambda t: jnp.flip(t, axis=1)
    o_b, s_b = gated_delta_chunked(fl(q), fl(k), fl(v), fl(g[:, :, 1]), fl(beta[:, :, 1]), s0_bwd)
    return o_f + fl(o_b), s_f, s_b


def gated_head_norm(o, z, norm_g):
    bsz, n = z.shape[0], z.shape[1]
    on = o * lax.rsqrt(jnp.mean(o * o, axis=-1, keepdims=True) + EPS) * norm_g.astype(jnp.float32)
    gate = jax.nn.silu(z.astype(jnp.float32)).reshape(bsz, n, DN_HEADS, DN_DV)
    return (on * gate).reshape(bsz, n, DN_WIDTH).astype(z.dtype)


def conformer_module(cf_in, conv_fn, dw_w, dw_b, ln_g, ln_b):
    y = cf_in[..., :CF_CH] * jax.nn.sigmoid(cf_in[..., CF_CH:])
    y = conv_fn(y, dw_w) + dw_b
    return jax.nn.silu(layernorm(y, ln_g, ln_b))


def swiglu(h, wg, wu, wd):
    return (jax.nn.silu(h @ wg) * (h @ wu)) @ wd


def moe_ffn(h, router_w, router_bias, w_gate, w_up, w_down, s_gate, s_up, s_down):
    bsz, n, d = h.shape
    t = bsz * n
    hf = h.reshape(t, d)
    scores = jax.nn.sigmoid((hf @ router_w).astype(jnp.float32))
    sel = scores + router_bias.astype(jnp.float32)
    grp_score = lax.top_k(sel.reshape(t, N_GROUPS, N_EXPERTS // N_GROUPS), 2)[0].sum(-1)
    _, grp_idx = lax.top_k(grp_score, TOPK_GROUPS)
    grp_keep = jnp.any(grp_idx[..., None] == jnp.arange(N_GROUPS), axis=-2)
    exp_keep = jnp.repeat(grp_keep, N_EXPERTS // N_GROUPS, axis=-1)
    _, top_idx = lax.top_k(jnp.where(exp_keep, sel, -jnp.inf), TOP_K)
    top_w = jnp.take_along_axis(scores, top_idx, axis=-1)
    top_w = top_w / jnp.sum(top_w, axis=-1, keepdims=True) * ROUTED_SCALE
    tk = t * TOP_K
    flat_e = top_idx.reshape(tk)
    order = jnp.argsort(flat_e, stable=True)
    sorted_e = flat_e[order]
    sorted_tok = (order // TOP_K).astype(jnp.int32)
    sorted_w = top_w.reshape(tk)[order]
    counts = jnp.bincount(flat_e, length=N_EXPERTS)
    padded = (counts + MOE_BLOCK - 1) // MOE_BLOCK * MOE_BLOCK
    start = jnp.cumsum(counts) - counts
    pad_end = jnp.cumsum(padded)
    pad_start = pad_end - padded
    dest = pad_start[sorted_e] + jnp.arange(tk) - start[sorted_e]
    n_blocks = -(-tk // MOE_BLOCK) + N_EXPERTS
    n_slots = n_blocks * MOE_BLOCK
    slot_tok = jnp.full((n_slots,), t, jnp.int32).at[dest].set(sorted_tok)
    slot_w = jnp.zeros((n_slots,), jnp.float32).at[dest].set(sorted_w)
    block_exp = jnp.minimum(jnp.searchsorted(pad_end, jnp.arange(n_blocks) * MOE_BLOCK, side='right'),
                            N_EXPERTS - 1)
    h_pad = jnp.concatenate([hf, jnp.zeros((1, d), hf.dtype)], axis=0)

    def expert_block(args):
        tok, e = args
        return swiglu(h_pad[tok], w_gate[e], w_up[e], w_down[e])

    out = lax.map(expert_block, (slot_tok.reshape(n_blocks, MOE_BLOCK), block_exp))
    weighted = (out.reshape(n_slots, d) * slot_w[:, None]).astype(h.dtype)
    routed = jnp.zeros((t + 1, d), h.dtype).at[slot_tok].add(weighted)[:t]
    return (routed + swiglu(hf, s_gate, s_up, s_down)).reshape(bsz, n, d)


def setup_inputs(seed: int = 0) -> dict:
    key = jax.random.key(seed)
    ks = jax.random.split(key, 32)
    f32 = jnp.float32
    L = DEPTH

    def nrm(k, shape, s):
        return jax.random.normal(k, shape, f32) * s

    dt = jnp.exp(jax.random.uniform(ks[11], (L, 2, DN_HEADS), f32, math.log(1e-3), math.log(1e-1)))
    return {
        'x': nrm(ks[0], (BATCH, SEQ, D_MODEL), 1.0),
        'c': nrm(ks[1], (BATCH, D_MODEL), 1.0),
        'ctx': nrm(ks[2], (BATCH, CTX_LEN, D_MODEL), 1.0),
        'c_ctx': nrm(ks[3], (D_MODEL,), 1.0),
        'w_mod': nrm(ks[4], (L, D_MODEL, 6 * D_MODEL), 0.5 * D_MODEL ** -0.5),
        'b_mod': nrm(ks[5], (L, 6 * D_MODEL), 0.02),
        'g_mix': 1.0 + nrm(ks[6], (L, D_MODEL), 0.02),
        'g_ffn': 1.0 + nrm(ks[7], (L, D_MODEL), 0.02),
        'w_in': nrm(ks[8], (L, D_MODEL, D_IN), D_MODEL ** -0.5),
        'w_out': nrm(ks[9], (L, D_MIX, D_MODEL), D_MIX ** -0.5),
        'dn_conv_w': nrm(ks[10], (L, SHORT_CONV, DN_CONV_DIM), SHORT_CONV ** -0.5),
        'dn_a_log': jnp.log(jax.random.uniform(ks[12], (L, 2, DN_HEADS), f32, 1.0, 16.0)),
        'dn_dt_bias': dt + jnp.log(-jnp.expm1(-dt)),
        'dn_norm_g': 1.0 + nrm(ks[13], (L, DN_DV), 0.02),
        'cf_dw_w': nrm(ks[14], (L, CF_K, CF_CH), CF_K ** -0.5),
        'cf_dw_b': nrm(ks[15], (L, CF_CH), 0.02),
        'cf_ln_g': 1.0 + nrm(ks[16], (L, CF_CH), 0.02),
        'cf_ln_b': nrm(ks[17], (L, CF_CH), 0.02),
        'router_w': nrm(ks[18], (L, D_MODEL, N_EXPERTS), D_MODEL ** -0.5),
        'router_bias': nrm(ks[19], (L, N_EXPERTS), 0.01),
        'exp_w_gate': nrm(ks[20], (L, N_EXPERTS, D_MODEL, D_EXPERT), D_MODEL ** -0.5),
        'exp_w_up': nrm(ks[21], (L, N_EXPERTS, D_MODEL, D_EXPERT), D_MODEL ** -0.5),
        'exp_w_down': nrm(ks[22], (L, N_EXPERTS, D_EXPERT, D_MODEL), D_EXPERT ** -0.5),
        'sh_w_gate': nrm(ks[23], (L, D_MODEL, D_SHARED), D_MODEL ** -0.5),
        'sh_w_up': nrm(ks[24], (L, D_MODEL, D_SHARED), D_MODEL ** -0.5),
        'sh_w_down': nrm(ks[25], (L, D_SHARED, D_MODEL), D_SHARED ** -0.5),
        'g_final': 1.0 + nrm(ks[26], (D_MODEL,), 0.02),
    }


def reference(x, c, ctx, c_ctx, w_mod, b_mod, g_mix, g_ffn, w_in, w_out, dn_conv_w, dn_a_log, dn_dt_bias,
              dn_norm_g, cf_dw_w, cf_dw_b, cf_ln_g, cf_ln_b, router_w, router_bias, exp_w_gate, exp_w_up,
              exp_w_down, sh_w_gate, sh_w_up, sh_w_down, g_final):
    bsz = x.shape[0]
    for l in range(DEPTH):
        sh1, sc1, gt1, sh2, sc2, gt2 = jnp.split((jax.nn.silu(c) @ w_mod[l] + b_mod[l])[:, None, :], 6, axis=-1)
        csh1, csc1, cgt1, csh2, csc2, cgt2 = jnp.split(jax.nn.silu(c_ctx) @ w_mod[l] + b_mod[l], 6, axis=-1)

        qkv_c, z_c, ba_c, cf_c = split_mixer_inputs(modulate(rmsnorm(ctx, g_mix[l]), csh1, csc1) @ w_in[l])
        s0 = jnp.zeros((bsz, DN_HEADS, DN_DK, DN_DV), jnp.float32)
        o_c, s_fwd, s_bwd = bidir_gated_delta(
            *delta_inputs(qkv_c, ba_c, dn_conv_w[l], dn_a_log[l], dn_dt_bias[l]), s0, s0)

        qkv_x, z_x, ba_x, cf_x = split_mixer_inputs(modulate(rmsnorm(x, g_mix[l]), sh1, sc1) @ w_in[l])
        o_x, _, _ = bidir_gated_delta(
            *delta_inputs(qkv_x, ba_x, dn_conv_w[l], dn_a_log[l], dn_dt_bias[l]), s_fwd, s_bwd)
        heads_x = jnp.concatenate([
            gated_head_norm(o_x, z_x, dn_norm_g[l]),
            conformer_module(cf_x, depthwise_conv_grid_columns, cf_dw_w[l], cf_dw_b[l], cf_ln_g[l], cf_ln_b[l]),
        ], axis=-1)
        x = x + gt1 * (heads_x @ w_out[l])
        x = x + gt2 * moe_ffn(modulate(rmsnorm(x, g_ffn[l]), sh2, sc2), router_w[l], router_bias[l],
                              exp_w_gate[l], exp_w_up[l], exp_w_down[l], sh_w_gate[l], sh_w_up[l], sh_w_down[l])

        if l + 1 < DEPTH:
            heads_c = jnp.concatenate([
                gated_head_norm(o_c, z_c, dn_norm_g[l]),
                conformer_module(cf_c, depthwise_conv_seq, cf_dw_w[l], cf_dw_b[l], cf_ln_g[l], cf_ln_b[l]),
            ], axis=-1)
            ctx = ctx + cgt1 * (heads_c @ w_out[l])
            ctx = ctx + cgt2 * moe_ffn(modulate(rmsnorm(ctx, g_ffn[l]), csh2, csc2), router_w[l], router_bias[l],
                                       exp_w_gate[l], exp_w_up[l], exp_w_down[l],
                                       sh_w_gate[l], sh_w_up[l], sh_w_down[l])
    return rmsnorm(x, g_final)
```

```python
from contextlib import ExitStack
import numpy as np
import concourse.bass as bass
import concourse.mybir as mybir
from concourse.bass_utils import run_bass_kernel_spmd

F32 = mybir.dt.float32
BF16 = mybir.dt.bfloat16
I32 = mybir.dt.int32
U32 = mybir.dt.uint32
AF = mybir.ActivationFunctionType
ALU = mybir.AluOpType
AX = mybir.AxisListType


class Lane:
    def __init__(self, sem, name):
        self.sem = sem
        self.count = 0
        self.name = name


class Sched:
    ENGS = ("pe", "act", "dve", "pool", "sp")

    def __init__(self, nc, stack):
        self.nc = nc
        self.stack = stack
        self.ops = {e: [] for e in self.ENGS}
        self.cnt = {e: 0 for e in self.ENGS}
        self.sem = {e: stack.enter_context(nc.semaphore("s_" + e)) for e in self.ENGS}
        self.waited = {e: {} for e in self.ENGS}
        self.last_w = {}
        self.readers = {}
        self.lanes = []
        self.n_wait = 0
        self.n_op = 0
        self.E = {"pe": nc.tensor, "act": nc.scalar, "dve": nc.vector, "pool": nc.gpsimd, "sp": nc.sync}

    def lane(self, name):
        ln = Lane(self.stack.enter_context(self.nc.semaphore("l_" + name)), name)
        self.lanes.append(ln)
        return ln

    def _need(self, eng, prod, val, needs):
        if prod is None:
            return
        cur = needs.get(prod, 0)
        if val > cur:
            needs[prod] = val

    def _deps(self, eng, reads, writes, self_prod):
        needs = {}
        for k in reads:
            lw = self.last_w.get(k)
            if lw is not None:
                self._need(eng, lw[0], lw[1], needs)
        same_ok = (self_prod == "pe") or not isinstance(self_prod, str)
        for k in writes:
            lw = self.last_w.get(k)
            if lw is not None and (lw[0] != self_prod or not same_ok):
                self._need(eng, lw[0], lw[1], needs)
            for p, v in self.readers.get(k, {}).items():
                if p != self_prod or not same_ok:
                    self._need(eng, p, v, needs)
        return needs

    def _emit_waits(self, eng, needs):
        w = self.waited[eng]
        for prod, val in needs.items():
            if w.get(prod, 0) >= val:
                continue
            w[prod] = val
            sem = self.sem[prod] if isinstance(prod, str) else prod.sem
            self.E[eng].wait_ge(sem, val)
            self.n_wait += 1

    def _record(self, prod, val, reads, writes):
        for k in writes:
            self.last_w[k] = (prod, val)
            self.readers[k] = {}
        for k in reads:
            r = self.readers.setdefault(k, {})
            if r.get(prod, 0) < val:
                r[prod] = val

    limit = None

    def op(self, eng, fn, reads=(), writes=()):
        if self.limit is not None and self.n_op >= self.limit:
            return
        needs = self._deps(eng, reads, writes, eng)
        self._emit_waits(eng, needs)
        self.cnt[eng] += 1
        val = self.cnt[eng]
        fn(self.E[eng]).then_inc(self.sem[eng], 1)
        self.n_op += 1
        self._record(eng, val, reads, writes)

    def dma(self, eng, lane, fns, reads=(), writes=()):
        needs = self._deps(eng, reads, writes, lane)
        if lane.count > 0:
            self._need(eng, lane, lane.count, needs)
        self._emit_waits(eng, needs)
        for fn in fns:
            lane.count += 16
            fn(self.E[eng]).then_inc(lane.sem, 16)
        self._record(lane, lane.count, reads, writes)

    def wait_all(self, eng, lanes):
        for ln in lanes:
            if ln.count > 0:
                self._emit_waits(eng, {ln: ln.count})

    def barrier(self):
        for e in self.ENGS:
            needs = {p: self.cnt[p] for p in self.ENGS if p != e and self.cnt[p] > 0}
            for ln in self.lanes:
                if ln.count > 0:
                    needs[ln] = ln.count
            self._emit_waits(e, needs)


HG = 2
NEG = -30000.0
EPS = 1e-6


def build_nc(nseq=2, stage=99, dumps=(), dn_limit=None):
    nc = bass.Bass("TRN2", target_bir_lowering=False)

    def din(name, shape, dt=F32):
        return nc.dram_tensor(name, shape, dt, kind="ExternalInput").ap()

    x_d = din("x", [2, 2048, 1024]); ctx_d = din("ctx", [2, 256, 1024])
    cT_d = din("cT", [128, 8, 4]); wmod_d = din("w_mod", [1024, 6144]); bmod_d = din("b_modT", [128, 48])
    gvec_d = din("gvec", [128, 3, 8]); win_d = din("w_in", [1024, 3088]); wout_d = din("w_out", [1024, 1024])
    convw_d = din("convw", [128, 12, 7]); bac_d = din("bac", [128, 2, 256]); normg_d = din("normg", [128, 1])
    cfw_d = din("cfw", [128, 4, 31]); cfv_d = din("cfv", [128, 3, 4])
    rw_d = din("router_w", [1024, 64]); rb_d = din("rbias", [128, 64])
    wg_d = din("wg", [65, 1024, 256]); wu_d = din("wu", [65, 1024, 256]); wd_d = din("wd", [65, 256, 1024])
    masks_d = din("masks", [128, 12, 128]); negrep_d = din("negrep", [128, 4, HG * 128])
    out_d = nc.dram_tensor("out", [2, 2048, 1024], F32, kind="ExternalOutput").ap()
    x1_d = nc.dram_tensor("x1s", [2048, 1024], F32, kind="Internal").ap()
    hT_d = nc.dram_tensor("hTs", [1024, 2048], BF16, kind="Internal").ap()
    dump_d = {}

    st0 = ExitStack()
    S = Sched(nc, st0)

    uniq = [0]

    def sb(st, name, shape, dt):
        uniq[0] += 1
        return st.enter_context(nc.sbuf_tensor("sb%d_%s" % (uniq[0], name), shape, dt))

    def MM(out, lhsT, rhs, start, stop, r, w):
        S.op("pe", lambda e: e.matmul(out, lhsT=lhsT, rhs=rhs, start=start, stop=stop), r, w)

    def TR(out, in_, ident, r, w):
        S.op("pe", lambda e: e.transpose(out, in_, ident), r, w)

    def ACT(out, in_, func, r, w, bias=None, scale=None, accum=None):
        kw = {}
        if bias is not None:
            kw["bias"] = bias
        if scale is not None:
            kw["scale"] = scale
        if accum is not None:
            kw["accum_out"] = accum
        S.op("act", lambda e: e.activation(out=out, in_=in_, func=func, **kw), r, w)

    def TT(eng, out, in0, in1, op, r, w):
        S.op(eng, lambda e: e.tensor_tensor(out=out, in0=in0, in1=in1, op=op), r, w)

    def TS(eng, out, in0, s1, op0, r, w, s2=None, op1=None):
        if s2 is None:
            S.op(eng, lambda e: e.tensor_scalar(out=out, in0=in0, scalar1=s1, scalar2=None, op0=op0), r, w)
        else:
            S.op(eng, lambda e: e.tensor_scalar(out=out, in0=in0, scalar1=s1, scalar2=s2, op0=op0, op1=op1), r, w)

    def STT(out, in0, scalar, in1, op0, op1, r, w):
        S.op("dve", lambda e: e.scalar_tensor_tensor(out=out, in0=in0, scalar=scalar, in1=in1, op0=op0, op1=op1), r, w)

    def CP(eng, out, in_, r, w):
        if eng == "act":
            S.op("act", lambda e: e.activation(out=out, in_=in_, func=AF.Identity), r, w)
        else:
            S.op(eng, lambda e: e.tensor_copy(out=out, in_=in_), r, w)

    def RCP(out, in_, r, w):
        S.op("dve", lambda e: e.reciprocal(out=out, in_=in_), r, w)

    def MSET(eng, ap, val, w):
        S.op(eng, lambda e: e.memset(ap, val), (), w)

    def DMA(eng, lane, out, in_, r, w):
        S.dma(eng, lane, [lambda e: e.dma_start(out=out, in_=in_)], r, w)

    dump_lane = S.lane("dump")

    def dump(name, ap, shape, dt, key):
        if name not in dumps:
            return
        d = nc.dram_tensor("dbg_" + name, list(shape), dt, kind="ExternalOutput").ap()
        dump_d[name] = d
        DMA("sp", dump_lane, d, ap, [key], [])

    ps_tr = st0.enter_context(nc.psum_tensor("ps_tr", [128, 1024], BF16))
    P = [st0.enter_context(nc.psum_tensor("ps%d" % i, [128, 512], F32)) for i in range(7)]

    masks = sb(st0, "masks", [128, 12, 128], F32)
    negrep = sb(st0, "negrep", [128, 4, HG * 128], F32)
    identb = sb(st0, "identb", [128, 128], BF16)
    onesb = sb(st0, "onesb", [128, 128], BF16)
    modS = sb(st0, "modS", [128, 48, 4], F32)
    G1 = sb(st0, "G1", [128, 8, 4], F32)
    G2 = sb(st0, "G2", [128, 8, 4], F32)
    gvec = sb(st0, "gvec", [128, 3, 8], F32)
    bmod = sb(st0, "bmod", [128, 48], F32)
    rbias = sb(st0, "rbias", [128, 64], F32)
    bac = sb(st0, "bac", [128, 2, 256], F32)
    negA = sb(st0, "negA", [128, 256], F32)
    convw = sb(st0, "convw", [128, 12, 7], F32)
    cfw = sb(st0, "cfw", [128, 4, 31], F32)
    cfv = sb(st0, "cfv", [128, 3, 4], F32)
    normg = sb(st0, "normg", [128, 1], F32)
    rwb = sb(st0, "rwb", [128, 8, 64], BF16)
    identf = masks[:, 10, :]
    onesf = masks[:, 11, :]
    TRI = [masks[:, 0, :], masks[:, 1, :]]
    STR = [masks[:, 2, :], masks[:, 3, :]]
    CH = [masks[:, 8, :], masks[:, 9, :]]

    l_c = S.lane("consts")
    S.dma("sp", l_c, [
        lambda e: e.dma_start(out=masks[:], in_=masks_d),
        lambda e: e.dma_start(out=negrep[:], in_=negrep_d),
        lambda e: e.dma_start(out=gvec[:], in_=gvec_d),
        lambda e: e.dma_start(out=bmod[:], in_=bmod_d),
        lambda e: e.dma_start(out=rbias[:], in_=rb_d),
        lambda e: e.dma_start(out=bac[:], in_=bac_d),
        lambda e: e.dma_start(out=convw[:], in_=convw_d),
        lambda e: e.dma_start(out=cfw[:], in_=cfw_d),
        lambda e: e.dma_start(out=cfv[:], in_=cfv_d),
        lambda e: e.dma_start(out=normg[:], in_=normg_d),
    ], [], ["consts"])
    CP("dve", identb[:], identf, ["consts"], ["identb"])
    CP("dve", onesb[:], onesf, ["consts"], ["onesb"])
    ACT(negA[:], bac[:, 1, :], AF.Exp, ["consts"], ["negA"])
    TS("dve", negA[:], negA[:], -1.0, ALU.mult, ["negA"], ["negA"])

    with ExitStack() as stp:
        scT = sb(stp, "scT", [128, 8, 4], F32)
        wm = [sb(stp, "wm%d" % i, [128, 8, 1024], F32) for i in range(2)]
        rwf = sb(stp, "rwf", [128, 8, 64], F32)
        l_wm = [S.lane("wm0"), S.lane("wm1")]
        DMA("sp", l_c, scT[:], cT_d, [], ["scT"])
        DMA("sp", l_c, rwf[:], rw_d.rearrange("(k p) e -> p k e", p=128), [], ["rwf"])
        ACT(scT[:], scT[:], AF.Silu, ["scT"], ["scT"])
        CP("pool", rwb[:], rwf[:], ["rwf"], ["rwb"])
        wmv = wmod_d.rearrange("(k p) c -> p k c", p=128)
        pm = P[0][:, 0:192].rearrange("p (c j) -> p c j", j=4)
        for cb in range(6):
            b = cb % 2
            DMA("sp", l_wm[b], wm[b][:], wmv[:, :, cb * 1024:(cb + 1) * 1024], [], ["wm%d" % b])
            for j in range(8):
                ch = cb * 8 + j
                for k in range(8):
                    MM(pm[:, ch, :], wm[b][:, k, j * 128:(j + 1) * 128], scT[:, k, :], k == 0, k == 7,
                       ["wm%d" % b, "scT"], ["P0"])
        for j in range(3):
            TT("dve", modS[:, :, j], pm[:, :, j], bmod[:], ALU.add, ["P0", "consts"], ["modS"])
        for j in range(3):
            STT(G1[:, :, j], modS[:, 8:16, j], 1.0, gvec[:, 0, :], ALU.add, ALU.mult, ["modS", "consts"], ["G1"])
            STT(G2[:, :, j], modS[:, 32:40, j], 1.0, gvec[:, 1, :], ALU.add, ALU.mult, ["modS", "consts"], ["G2"])
    S.barrier()
    dump("modS", modS[:], [128, 48, 4], F32, "modS")
    if stage <= 0:
        S.barrier()
        st0.close()
        return nc, dump_d

    def row_bcast(dst, colsrc, tmp, keys_r, key_w, tkey="rb_tmp"):
        for half in range(2):
            for kk in range(4):
                k = half * 4 + kk
                TS("dve", tmp[:, kk, :], identf, colsrc(k), ALU.mult, keys_r + ["consts"], [tkey])
                MM(P[1][:, kk * 128:(kk + 1) * 128], onesf, tmp[:, kk, :], True, True, [tkey, "consts"], ["P1"])
            CP("act", dst[:, half * 512:(half + 1) * 512], P[1][:, :], ["P1"], [key_w])


    for s in range(nseq):
        stA = ExitStack()
        xnT = sb(stA, "xnT", [128, 8, 2048], BF16)
        cfT = sb(stA, "cfT", [128, 4, 2048], BF16)
        dnT = sb(stA, "dnT", [128, 4, 2048], BF16)
        qT = sb(stA, "qT", [128, HG, 2048], BF16)
        kT = sb(stA, "kT", [128, HG, 2048], BF16)
        cvT = sb(stA, "cvT", [128, HG, 2048], BF16)
        zsT = sb(stA, "zsT", [128, HG, 2048], BF16)
        ostore = sb(stA, "ostore", [128, 16, HG, 128], F32)
        ubuf = sb(stA, "ubuf", [128, 2056], BF16)
        ybuf = sb(stA, "ybuf", [128, 3968], BF16)
        wst = [sb(stA, "wst%d" % i, [128, 8, 128], F32) for i in range(2)]
        wcb = [sb(stA, "wcb%d" % i, [128, 8, 128], BF16) for i in range(2)]
        dgq = sb(stA, "dgq", [128, 7, 128], BF16)
        dgc = sb(stA, "dgc", [128, 31, 128], BF16)
        xin = sb(stA, "xin", [128, 1024], F32)
        xs = sb(stA, "xs", [128, 1024], BF16)
        junk = sb(stA, "junk", [128, 1024], BF16)
        st1 = sb(stA, "st1", [128, 8], F32)
        tmpa = sb(stA, "tmpa", [128, 512], F32)
        tmpb = sb(stA, "tmpb", [128, 512], F32)
        tmpc = sb(stA, "tmpc", [128, 512], BF16)
        gall = sb(stA, "gall", [128, 288], F32)
        ball = sb(stA, "ball", [128, 288], F32)
        nball = sb(stA, "nball", [128, 288], F32)
        xnTc = sb(stA, "xnTc", [128, 8, 256], BF16)
        baA = sb(stA, "baA", [128, 256], F32)
        wbab = sb(stA, "wbab", [128, 8, 16], BF16)
        wbaf = sb(stA, "wbaf", [128, 8, 16], F32)
        l_x = S.lane("xin%d" % s)
        l_w = [S.lane("wst%d_%d" % (s, i)) for i in range(2)]
        wctr = [0]

        MSET("pool", ubuf[:], 0.0, ["ubuf"])
        MSET("pool", ybuf[:], 0.0, ["ybuf"])

        def norm_mod_T(src, skey, j, Gm, shbase, dstT, col0, key_dst):
            ACT(junk[:], src, AF.Square, [skey], ["junk", "st1a"], accum=st1[:, 0:1])
            ACT(st1[:, 1:2], st1[:, 0:1], AF.Sqrt, ["st1a"], ["st1b"], bias=EPS, scale=1.0 / 1024)
            RCP(st1[:, 2:3], st1[:, 1:2], ["st1b"], ["st1c"])
            TS("dve", xs[:], src, st1[:, 2:3], ALU.mult, [skey, "st1c"], ["xs"])
            for k in range(8):
                TR(ps_tr[:, k * 128:(k + 1) * 128], xs[:, k * 128:(k + 1) * 128], identb[:], ["xs", "identb"], ["ps_tr"])
            for k in range(8):
                if k % 2 == 0:
                    ACT(dstT[:, k, col0:col0 + 128], ps_tr[:, k * 128:(k + 1) * 128], AF.Identity,
                        ["ps_tr", "G1", "G2", "modS"], [key_dst], bias=modS[:, shbase + k, j:j + 1], scale=Gm[:, k, j:j + 1])
                else:
                    TS("dve", dstT[:, k, col0:col0 + 128], ps_tr[:, k * 128:(k + 1) * 128], Gm[:, k, j:j + 1], ALU.mult,
                       ["ps_tr", "G1", "G2", "modS"], [key_dst], s2=modS[:, shbase + k, j:j + 1], op1=ALU.add)

        def load_w(col0, ncols=128):
            b = wctr[0] % 2
            wctr[0] += 1
            DMA("sp", l_w[b], wst[b][:, :, 0:ncols], win_d.rearrange("(k p) c -> p k c", p=128)[:, :, col0:col0 + ncols],
                [], ["wst%d" % b])
            CP("pool", wcb[b][:, :, 0:ncols], wst[b][:, :, 0:ncols], ["wst%d" % b], ["wcb%d" % b])
            return wcb[b], "wcb%d" % b

        def proj_tile(wt, wkey, xsrc, xkey, t0, tw, pbank, pkey):
            for k in range(8):
                MM(pbank[:, 0:tw], wt[:, k, :], xsrc[:, k, t0:t0 + tw], k == 0, k == 7, [wkey, xkey], [pkey])

        def l2norm_store(src_bf, dst, scale, bias):
            tw = src_bf.shape[1]
            TT("dve", tmpc[:, 0:tw], src_bf, src_bf, ALU.mult, ["cs"], ["tmpc"])
            MM(P[3][:, 0:tw], onesb[:], tmpc[:, 0:tw], True, True, ["tmpc", "onesb"], ["P3"])
            ACT(tmpa[:, 0:tw], P[3][:, 0:tw], AF.Sqrt, ["P3"], ["tmpa"], bias=bias, scale=scale)
            RCP(tmpb[:, 0:tw], tmpa[:, 0:tw], ["tmpa"], ["tmpb"])
            TT("dve", dst, src_bf, tmpb[:, 0:tw], ALU.mult, ["cs", "tmpb"], ["qkv"])

        def conv_chunk(cc, N, kind, dst, xsrc, xkey):
            wt, wkey = load_w(cc * 128)
            tw = min(512, N)
            for t in range(N // tw):
                proj_tile(wt, wkey, xsrc, xkey, t * tw, tw, P[1], "P1")
                CP("dve", ubuf[:, 4 + t * tw:4 + (t + 1) * tw], P[1][:, 0:tw], ["P1"], ["ubuf"])
            if N < 2048:
                MSET("pool", ubuf[:, 4 + N:8 + N], 0.0, ["ubuf"])
            for jt in range(7):
                TS("dve", dgq[:, jt, :], identb[:], convw[:, cc, jt:jt + 1], ALU.mult, ["identb", "consts"], ["dgq"])
            cs = sb_cs
            for t in range(N // tw):
                for jt in range(7):
                    MM(P[2][:, 0:tw], dgq[:, jt, :], ubuf[:, t * tw + jt + 1:t * tw + jt + 1 + tw], jt == 0, jt == 6, ["dgq", "ubuf"], ["P2"])
                if kind == "v":
                    ACT(dst[:, t * tw:(t + 1) * tw], P[2][:, 0:tw], AF.Silu, ["P2"], ["qkv"])
                else:
                    ACT(cs[:, 0:tw], P[2][:, 0:tw], AF.Silu, ["P2"], ["cs"])
                    if kind == "q":
                        l2norm_store(cs[:, 0:tw], dst[:, t * tw:(t + 1) * tw], 128.0, 128.0 * EPS)
                    else:
                        l2norm_store(cs[:, 0:tw], dst[:, t * tw:(t + 1) * tw], 1.0, EPS)

        sb_cs = sb(stA, "cs", [128, 512], BF16)

        def ba_all(N, xsrc, xkey, tb):
            nt = N // 128
            DMA("sp", l_x, wbaf[:], win_d.rearrange("(k p) c -> p k c", p=128)[:, :, 2048:2064], [], ["wbaf"])
            CP("pool", wbab[:], wbaf[:], ["wbaf"], ["wbab"])
            for i in range(nt):
                for k in range(8):
                    MM(P[1][:, i * 16:(i + 1) * 16], xsrc[:, k, i * 128:(i + 1) * 128], wbab[:, k, :], k == 0, k == 7,
                       [xkey, "wbab"], ["P1"])
            w = nt * 16
            o = tb * 16
            TT("dve", baA[:, 0:w], P[1][:, 0:w], bac[:, 0, 0:w], ALU.add, ["P1", "consts"], ["baA"])
            ACT(baA[:, 0:w], baA[:, 0:w], AF.Exp, ["baA"], ["baA"])
            ACT(baA[:, 0:w], baA[:, 0:w], AF.Ln, ["baA"], ["baA"], bias=1.0)
            TT("dve", gall[:, o:o + w], baA[:, 0:w], negA[:, 0:w], ALU.mult, ["baA", "negA"], ["gall"])
            ACT(ball[:, o:o + w], P[1][:, 0:w], AF.Sigmoid, ["P1"], ["ball"])
            TS("dve", nball[:, o:o + w], ball[:, o:o + w], -1.0, ALU.mult, ["ball"], ["nball"])

        exs = sb(stA, "exs", [128, 16], F32)
        bg = sb(stA, "bg", [128, HG], F32)
        gtri = sb(stA, "gtri", [128, HG * 128], F32)
        decs = sb(stA, "decs", [128, HG * 128], BF16)
        dect = sb(stA, "dect", [128, HG * 128], BF16)
        Pb = sb(stA, "Pb", [128, HG, 128], BF16)
        ATb = sb(stA, "ATb", [128, HG, 128], BF16)
        Qb = sb(stA, "Qb", [128, HG, 128], BF16)
        Tb = sb(stA, "Tb", [128, HG, 128], BF16)
        kdb = sb(stA, "kdb", [128, HG, 128], BF16)
        kbgb = sb(stA, "kbgb", [128, HG, 128], BF16)
        vbb = sb(stA, "vbb", [128, HG, 128], BF16)
        usb = sb(stA, "usb", [128, HG * 128], F32)
        wTb = sb(stA, "wTb", [128, HG, 128], BF16)
        vn = sb(stA, "vn", [128, HG, 128], BF16)
        osb = sb(stA, "osb", [128, HG * 128], F32)
        otmp = sb(stA, "otmp", [128, HG * 128], F32)
        Sb = [[sb(stA, "S%d_%d" % (d, h), [128, 128], BF16) for h in range(HG)] for d in range(2)]
        ont = sb(stA, "ont", [128, HG, 128], BF16)
        ms = sb(stA, "ms", [128, 8], F32)
        MSET("pool", vn[:], 0.0, ["vn"])

        def dn_tile(g, i, d, lat):
            if dn_limit is not None and S.limit is None:
                S.limit = S.n_op + dn_limit
            tc0 = i * 128
            ib = i if lat else 16 + i
            gcol = ib * 16 + d * 8 + 4 + g * HG
            bcol = ib * 16 + d * 8 + g * HG
            g4 = ib * 16 + d * 8 + 4
            MM(P[0][:, 0:4], TRI[d], gall[:, g4:g4 + 4], True, True, ["consts", "gall"], ["P0"])
            MM(P[0][:, 4:8], CH[0], gall[:, g4:g4 + 4], True, True, ["consts", "gall"], ["P0"])
            MM(P[0][:, 8:12], CH[1], gall[:, g4:g4 + 4], True, True, ["consts", "gall"], ["P0"])
            MM(P[0][:, 12:16], STR[d], gall[:, g4:g4 + 4], True, True, ["consts", "gall"], ["P0"])
            ACT(exs[:, 0:16], P[0][:, 0:16], AF.Exp, ["P0"], ["exs"])
            e0 = g * HG
            TT("dve", bg[:], exs[:, e0:e0 + 2], ball[:, bcol:bcol + 2], ALU.mult, ["exs", "ball"], ["bg"])
            for h in range(HG):
                TS("pool", gtri[:, h * 128:(h + 1) * 128], TRI[d], gall[:, gcol + h:gcol + h + 1], ALU.mult,
                   ["consts", "gall"], ["gtri"])
            MM(P[1][:, 0:HG * 128], identf, negrep[:, d, :], True, False, ["consts"], ["P1"])
            for h in range(HG):
                MM(P[1][:, h * 128:(h + 1) * 128], gtri[:, h * 128:(h + 1) * 128], STR[d], False, h == HG - 1, ["gtri", "consts"], ["P1"])
            MM(P[2][:, 0:HG * 128], identf, negrep[:, 2 + d, :], True, False, ["consts"], ["P2"])
            MM(P[2][:, 0:HG * 128], STR[d], gtri[:], False, True, ["gtri", "consts"], ["P2"])
            ACT(decs[:], P[1][:, 0:HG * 128], AF.Exp, ["P1"], ["decs"])
            ACT(dect[:], P[2][:, 0:HG * 128], AF.Exp, ["P2"], ["dect"])
            for h in range(HG):
                MM(P[3][:, h * 128:(h + 1) * 128], kT[:, h, tc0:tc0 + 128], kT[:, h, tc0:tc0 + 128], True, True, ["qkv"], ["P3"])
            if lat:
                for h in range(HG):
                    MM(P[3][:, 256 + h * 128:256 + (h + 1) * 128], kT[:, h, tc0:tc0 + 128], qT[:, h, tc0:tc0 + 128], True, True, ["qkv"], ["P3"])
            for h in range(HG):
                STT(Pb[:, h, :], P[3][:, h * 128:(h + 1) * 128], nball[:, bcol + h:bcol + h + 1], decs[:, h * 128:(h + 1) * 128],
                    ALU.mult, ALU.mult, ["P3", "nball", "decs"], ["Pb"])
            if lat:
                TT("dve", ATb[:].rearrange("p h c -> p (h c)"), P[3][:, 256:512], dect[:], ALU.mult, ["P3", "dect"], ["ATb"])
            for h in range(HG):
                TR(ps_tr[:, h * 128:(h + 1) * 128], kT[:, h, tc0:tc0 + 128], identb[:], ["qkv", "identb"], ["ps_tr"])
                TR(ps_tr[:, 256 + h * 128:256 + (h + 1) * 128], cvT[:, h, tc0:tc0 + 128], identb[:], ["qkv", "identb"], ["ps_tr"])
                TR(ps_tr[:, 512 + h * 128:512 + (h + 1) * 128], Pb[:, h, :], identb[:], ["Pb", "identb"], ["ps_tr"])
            for h in range(HG):
                ACT(kdb[:, h, :], ps_tr[:, h * 128:(h + 1) * 128], AF.Identity, ["ps_tr", "exs"], ["kdb"], scale=exs[:, 12 + e0 + h:13 + e0 + h])
                ACT(kbgb[:, h, :], ps_tr[:, h * 128:(h + 1) * 128], AF.Identity, ["ps_tr", "bg"], ["kbgb"], scale=bg[:, h:h + 1])
                ACT(vbb[:, h, :], ps_tr[:, 256 + h * 128:256 + (h + 1) * 128], AF.Identity, ["ps_tr", "ball"], ["vbb"],
                    scale=ball[:, bcol + h:bcol + h + 1])
                CP("act", Qb[:, h, :], ps_tr[:, 512 + h * 128:512 + (h + 1) * 128], ["ps_tr"], ["QTq"])
                CP("pool", Tb[:, h, :], identb[:], ["identb"], ["QTt"])
            p4 = P[4][:, :].rearrange("p (h t c) -> p h t c", h=HG, t=2)
            p5 = P[5][:, 0:HG * 128].rearrange("p (h c) -> p h c", h=HG)
            for m in range(5):
                for h in range(HG):
                    MM(P[4][:, h * 256:h * 256 + 128], Pb[:, h, :], Qb[:, h, :], True, True, ["Pb", "QTq"], ["P4"])
                    MM(P[4][:, h * 256 + 128:(h + 1) * 256], Pb[:, h, :], Tb[:, h, :], True, True, ["Pb", "QTt"], ["P4"])
                for h in range(HG):
                    MM(P[5][:, h * 128:(h + 1) * 128], Qb[:, h, :], Pb[:, h, :], True, True, ["Pb", "QTq"], ["P5"])
                TT("dve", Tb[:], Tb[:], p4[:, :, 1, :], ALU.add, ["QTt", "P4"], ["QTt"])
                for h in range(HG):
                    CP("dve", Qb[:, h, :], P[4][:, h * 256:h * 256 + 128], ["P4"], ["QTq"])
                CP("dve", Pb[:].rearrange("p h c -> p (h c)"), P[5][:, 0:HG * 128], ["P5"], ["Pb"])
            for h in range(HG):
                MM(P[5][:, h * 128:(h + 1) * 128], Pb[:, h, :], Tb[:, h, :], True, True, ["Pb", "QTt"], ["P5"])
            TT("dve", Tb[:], Tb[:], p5, ALU.add, ["QTt", "P5"], ["QTt"])
            for h in range(HG):
                MM(P[3][:, h * 128:(h + 1) * 128], Tb[:, h, :], vbb[:, h, :], True, True, ["QTt", "vbb"], ["P3"])
                MM(P[5][:, h * 128:(h + 1) * 128], kbgb[:, h, :], Tb[:, h, :], True, True, ["QTt", "kbgb"], ["P5"])
            CP("dve", usb[:], P[3][:, 0:HG * 128], ["P3"], ["usb"])
            CP("dve", wTb[:].rearrange("p h c -> p (h c)"), P[5][:, 0:HG * 128], ["P5"], ["wTb"])
            for c in ([] if stage == 0.85 else ([0, 1] if d == 0 else [1, 0])):
                pr = slice(c * 64, (c + 1) * 64)
                for h in range(HG):
                    MM(P[6][pr, h * 128:(h + 1) * 128], wTb[:, h, pr], Sb[d][h][:], True, True, ["wTb", "S%d%d" % (d, h)], ["P6"])
                if lat:
                    for h in range(HG):
                        MM(P[6][pr, 256 + h * 128:256 + (h + 1) * 128], qT[:, h, tc0 + c * 64:tc0 + (c + 1) * 64], Sb[d][h][:], True, True,
                           ["qkv", "S%d%d" % (d, h)], ["P6"])
                TT("dve", vn[pr].rearrange("p h c -> p (h c)"), usb[pr, :], P[6][pr, 0:HG * 128], ALU.subtract, ["usb", "P6"], ["vn"])
                if lat:
                    for h in range(HG):
                        TS("dve", osb[pr, h * 128:(h + 1) * 128], P[6][pr, 256 + h * 128:256 + (h + 1) * 128], exs[pr, e0 + h:e0 + h + 1],
                           ALU.mult, ["P6", "exs"], ["osb"])
                    for h in range(HG):
                        MM(P[3][pr, 256 + h * 128:256 + (h + 1) * 128], ATb[pr, h, pr], vn[pr, h, :], True, True, ["ATb", "vn"], ["P3"])
                for h in range(HG):
                    MM(P[0][:, 256 + h * 128:256 + (h + 1) * 128], kdb[pr, h, :], vn[pr, h, :], True, True, ["kdb", "vn"], ["P0"])
                if lat:
                    oview = ostore[pr, i, :, :].rearrange("p h c -> p (h c)")
                    if d == 0:
                        TT("dve", oview, osb[pr, :], P[3][pr, 256:512], ALU.add, ["osb", "P3"], ["ostore"])
                    else:
                        TT("dve", otmp[pr, :], osb[pr, :], P[3][pr, 256:512], ALU.add, ["osb", "P3"], ["otmp"])
                        TT("pool", oview, oview, otmp[pr, :], ALU.add, ["otmp", "ostore"], ["ostore"])
                for h in range(HG):
                    STT(Sb[d][h][:], Sb[d][h][:], exs[:, 4 + 4 * c + e0 + h:5 + 4 * c + e0 + h], P[0][:, 256 + h * 128:256 + (h + 1) * 128],
                        ALU.mult, ALU.add, ["S%d%d" % (d, h), "exs", "P0"], ["S%d%d" % (d, h)])

        tmpd = sb(stA, "tmpd", [128, 512], F32)

        def conformer():
            for cc in range(4):
                wa, wak = load_w(2064 + cc * 128)
                wgt, wgk = load_w(2576 + cc * 128)
                for t in range(4):
                    proj_tile(wa, wak, xnT, "xnT", t * 512, 512, P[1], "P1")
                    proj_tile(wgt, wgk, xnT, "xnT", t * 512, 512, P[2], "P2")
                    ACT(tmpa[:], P[2][:, :], AF.Sigmoid, ["P2"], ["tmpa"])
                    TT("dve", ybuf[:, 960 + t * 512:960 + (t + 1) * 512], P[1][:, :], tmpa[:], ALU.mult, ["P1", "tmpa"], ["ybuf"])
                for jt in range(31):
                    TS("dve", dgc[:, jt, :], identb[:], cfw[:, cc, jt:jt + 1], ALU.mult, ["identb", "consts"], ["dgc"])
                for t in range(4):
                    for jt in range(31):
                        MM(P[1][:, :], dgc[:, jt, :], ybuf[:, t * 512 + jt * 64:t * 512 + jt * 64 + 512], jt == 0, jt == 30,
                           ["dgc", "ybuf"], ["P1"])
                    ACT(cfT[:, cc, t * 512:(t + 1) * 512], P[1][:, :], AF.Identity, ["P1", "consts"], ["cfT"], bias=cfv[:, 0, cc:cc + 1])
            for t in range(4):
                sl = slice(t * 512, (t + 1) * 512)
                for cc in range(4):
                    MM(P[5][:, :], onesb[:], cfT[:, cc, sl], cc == 0, cc == 3, ["onesb", "cfT"], ["P5"])
                for cc in range(4):
                    TT("dve", tmpc[:], cfT[:, cc, sl], cfT[:, cc, sl], ALU.mult, ["cfT"], ["tmpc"])
                    MM(P[6][:, :], onesb[:], tmpc[:], cc == 0, cc == 3, ["onesb", "tmpc"], ["P6"])
                TS("dve", tmpa[:], P[5][:, :], 1.0 / 512, ALU.mult, ["P5"], ["tmpa"])
                TT("dve", tmpb[:], tmpa[:], tmpa[:], ALU.mult, ["tmpa"], ["tmpb"])
                STT(tmpb[:], P[6][:, :], 1.0 / 512, tmpb[:], ALU.mult, ALU.subtract, ["P6", "tmpb"], ["tmpb"])
                ACT(tmpb[:], tmpb[:], AF.Sqrt, ["tmpb"], ["tmpb"], bias=EPS)
                RCP(tmpb[:], tmpb[:], ["tmpb"], ["tmpb"])
                for cc in range(4):
                    TT("dve", tmpd[:], cfT[:, cc, sl], tmpa[:], ALU.subtract, ["cfT", "tmpa"], ["tmpd"])
                    TT("dve", tmpd[:], tmpd[:], tmpb[:], ALU.mult, ["tmpd", "tmpb"], ["tmpd"])
                    ACT(cfT[:, cc, sl], tmpd[:], AF.Silu, ["tmpd", "consts"], ["cfT"], bias=cfv[:, 2, cc:cc + 1], scale=cfv[:, 1, cc:cc + 1])

        def dn_finish(g):
            for i in range(16):
                for h in range(HG):
                    ACT(junk[:, 0:128], ostore[:, i, h, :], AF.Square, ["ostore"], ["junk", "ms_a"], accum=ms[:, h:h + 1])
                ACT(ms[:, 2:4], ms[:, 0:2], AF.Sqrt, ["ms_a"], ["ms_b"], bias=EPS, scale=1.0 / 128)
                RCP(ms[:, 4:6], ms[:, 2:4], ["ms_b"], ["ms_c"])
                for h in range(HG):
                    TS("dve", ont[:, h, :], ostore[:, i, h, :], ms[:, 4 + h:5 + h], ALU.mult, ["ostore", "ms_c"], ["ont"])
                for h in range(HG):
                    TR(ps_tr[:, 768 + h * 128:768 + (h + 1) * 128], ont[:, h, :], identb[:], ["ont", "identb"], ["ps_tr"])
                for h in range(HG):
                    STT(dnT[:, g * HG + h, i * 128:(i + 1) * 128], ps_tr[:, 768 + h * 128:768 + (h + 1) * 128], normg[:, 0:1],
                        zsT[:, h, i * 128:(i + 1) * 128], ALU.mult, ALU.mult, ["ps_tr", "consts", "zsT"], ["dnT"])

        for i in range(2):
            DMA("sp", l_x, xin[:], ctx_d[s, i * 128:(i + 1) * 128, :], [], ["xin"])
            norm_mod_T(xin[:], "xin", 2, G1, 0, xnTc, i * 128, "xnTc")
        for i in range(16):
            DMA("sp", l_x, xin[:], x_d[s, i * 128:(i + 1) * 128, :], [], ["xin"])
            norm_mod_T(xin[:], "xin", s, G1, 0, xnT, i * 128, "xnT")
        dump("xnT", xnT[:], [128, 8, 2048], BF16, "xnT")
        if stage == 0.5:
            S.barrier(); stA.close(); continue
        ba_all(256, xnTc, "xnTc", 16)
        ba_all(2048, xnT, "xnT", 0)
        dump("gall", gall[:], [128, 288], F32, "gall")
        dump("ball", ball[:], [128, 288], F32, "ball")
        if stage == 0.7:
            S.barrier(); stA.close(); continue
        if stage >= 2:
            conformer()
            dump("cfT", cfT[:], [128, 4, 2048], BF16, "cfT")
        for g in range(4 // HG):
            for d in range(2):
                for h in range(HG):
                    MSET("pool", Sb[d][h][:], 0.0, ["S%d%d" % (d, h)])
            for h in range(HG):
                conv_chunk(4 + g * HG + h, 256, "k", kT[:, h, 0:256], xnTc, "xnTc")
                conv_chunk(8 + g * HG + h, 256, "v", cvT[:, h, 0:256], xnTc, "xnTc")
            if stage == 0.8:
                dump("kTc", kT[:, :, 0:256], [128, HG, 256], BF16, "qkv")
                dump("cvTc", cvT[:, :, 0:256], [128, HG, 256], BF16, "qkv")
                S.barrier(); stA.close(); break
            for i in (0, 1):
                dn_tile(g, i, 0, False)
            for i in (1, 0):
                dn_tile(g, i, 1, False)
            if g == 0:
                dump("kTc", kT[:, :, 0:256], [128, HG, 256], BF16, "qkv")
                dump("Sctx", Sb[0][0][:], [128, 128], BF16, "S00")
            if stage in (0.9, 0.85):
                dump("wTb", wTb[:], [128, HG, 128], BF16, "wTb")
                dump("usb", usb[:], [128, HG * 128], F32, "usb")
                S.barrier(); stA.close(); break
            for h in range(HG):
                hh = g * HG + h
                wz, wzk = load_w(1536 + hh * 128)
                for t in range(4):
                    proj_tile(wz, wzk, xnT, "xnT", t * 512, 512, P[1], "P1")
                    ACT(zsT[:, h, t * 512:(t + 1) * 512], P[1][:, :], AF.Silu, ["P1"], ["zsT"])
                conv_chunk(hh, 2048, "q", qT[:, h, :], xnT, "xnT")
                conv_chunk(4 + hh, 2048, "k", kT[:, h, :], xnT, "xnT")
                conv_chunk(8 + hh, 2048, "v", cvT[:, h, :], xnT, "xnT")
            if g == 0:
                dump("qT", qT[:], [128, HG, 2048], BF16, "qkv")
                dump("kT", kT[:], [128, HG, 2048], BF16, "qkv")
                dump("cvT", cvT[:], [128, HG, 2048], BF16, "qkv")
            for i in range(16):
                dn_tile(g, i, 0, True)
            for i in range(15, -1, -1):
                dn_tile(g, i, 1, True)
            if g == 0:
                dump("ostore", ostore[:], [128, 16, HG, 128], F32, "ostore")
            dn_finish(g)
        dump("dnT", dnT[:], [128, 4, 2048], BF16, "dnT")
        if stage <= 2:
            S.barrier()
            stA.close()
            continue
        wo_h = [qT[:].rearrange("p a (b c) -> p (a b) c", c=512), kT[:].rearrange("p a (b c) -> p (a b) c", c=512)]
        gt1bc = sb(stA, "gt1bc", [128, 1024], F32)
        rbt = tmpd[:].rearrange("p (a c) -> p a c", c=128)
        xhalf = [tmpa, tmpb]
        hTt = [sb(stA, "hTt%d" % i, [128, 8, 128], BF16) for i in range(2)]
        l_x1 = S.lane("x1st%d" % s)
        l_h = [S.lane("hst%d_%d" % (s, i)) for i in range(2)]
        for c8 in range(8):
            b = wctr[0] % 2
            wctr[0] += 1
            DMA("sp", l_w[b], wst[b][:], wout_d.rearrange("(k p) c -> p k c", p=128)[:, :, c8 * 128:(c8 + 1) * 128], [], ["wst%d" % b])
            CP("pool", wo_h[c8 // 4][:, :, (c8 % 4) * 128:(c8 % 4 + 1) * 128], wst[b][:], ["wst%d" % b], ["qkv"])
        row_bcast(gt1bc, lambda k: modS[:, 16 + k, s:s + 1], rbt, ["modS", "tmpd"], "gt1bc", "tmpd")
        hTv = hT_d.rearrange("(k p) t -> p k t", p=128)
        for i in range(16):
            ts_ = slice(i * 128, (i + 1) * 128)
            DMA("sp", l_x, xin[:], x_d[s, ts_, :], [], ["xin"])
            for half in range(2):
                for k in range(8):
                    src = dnT[:, k, ts_] if k < 4 else cfT[:, k - 4, ts_]
                    MM(P[1 + half][:, :], src, wo_h[half][:, k, :], k == 0, k == 7,
                       ["dnT", "cfT", "qkv"], ["P%d" % (1 + half)])
                hk = "tmpa" if half == 0 else "tmpb"
                TT("dve", xhalf[half][:], P[1 + half][:, :], gt1bc[:, half * 512:(half + 1) * 512], ALU.mult,
                   ["P%d" % (1 + half), "gt1bc"], [hk])
                TT("pool", xin[:, half * 512:(half + 1) * 512], xin[:, half * 512:(half + 1) * 512], xhalf[half][:], ALU.add,
                   [hk, "xin"], ["xin"])
            DMA("sp", l_x1, x1_d[ts_, :], xin[:], ["xin"], ["x1_d"])
            b = i % 2
            norm_mod_T(xin[:], "xin", s, G2, 24, hTt[b], 0, "hTt%d" % b)
            DMA("sp", l_h[b], hTv[:, :, ts_], hTt[b][:], ["hTt%d" % b], ["hT_d"])
        S.barrier()
        stA.close()

        stM = ExitStack()
        hT = sb(stM, "hT", [128, 8, 2048], BF16)
        acc = sb(stM, "acc", [128, 16, 1024], F32)
        Wr = sb(stM, "Wr", [128, 16, 64], F32)
        l_m = S.lane("moe_in%d" % s)
        DMA("sp", l_m, hT[:], hTv, ["hT_d"], ["hT"])
        with ExitStack() as stR:
            scores = sb(stR, "scores", [128, 16, 64], F32)
            sel = sb(stR, "sel", [128, 16, 64], F32)
            sel2 = sb(stR, "sel2", [128, 16, 64], F32)
            m1 = sb(stR, "m1", [128, 128], F32)
            m2 = sb(stR, "m2", [128, 128], F32)
            top8 = sb(stR, "top8", [128, 16, 8], F32)
            keepg = sb(stR, "keepg", [128, 128], F32)
            nrm = sb(stR, "nrm", [128, 16], F32)
            for i in range(16):
                for k in range(8):
                    MM(P[0][:, (i % 8) * 64:(i % 8 + 1) * 64], hT[:, k, i * 128:(i + 1) * 128], rwb[:, k, :], k == 0, k == 7, ["hT", "rwb"], ["P0"])
                if i % 8 == 7:
                    ACT(scores[:, i - 7:i + 1, :].rearrange("p a e -> p (a e)"), P[0][:, :], AF.Sigmoid, ["P0"], ["scores"])
            TT("dve", sel[:], scores[:], rbias[:, :].unsqueeze(1).to_broadcast([128, 16, 64]), ALU.add, ["scores", "consts"], ["sel"])
            selg = sel[:].rearrange("p a (g e) -> p (a g) e", e=8)
            sel2g = sel2[:].rearrange("p a (g e) -> p (a g) e", e=8)
            S.op("dve", lambda e: e.tensor_reduce(out=m1[:], in_=selg, axis=AX.X, op=ALU.max), ["sel"], ["m1"])
            TT("dve", sel2g, selg, m1[:, :].unsqueeze(2).to_broadcast([128, 128, 8]), ALU.is_equal, ["sel", "m1"], ["sel2"])
            STT(sel2[:], sel2[:], -1e9, sel[:], ALU.mult, ALU.add, ["sel2", "sel"], ["sel2"])
            S.op("dve", lambda e: e.tensor_reduce(out=m2[:], in_=sel2g, axis=AX.X, op=ALU.max), ["sel2"], ["m2"])
            TT("dve", m1[:], m1[:], m2[:], ALU.add, ["m1", "m2"], ["m1"])
            for i in range(16):
                S.op("dve", lambda e: e.max(out=top8[:, i, :], in_=m1[:, i * 8:(i + 1) * 8]), ["m1"], ["top8"])
            TT("dve", keepg[:].rearrange("p (a g) -> p a g", g=8), m1[:].rearrange("p (a g) -> p a g", g=8),
               top8[:, :, 3:4].to_broadcast([128, 16, 8]), ALU.is_ge, ["m1", "top8"], ["keepg"])
            TS("dve", sel2[:], sel[:], 10.0, ALU.add, ["sel"], ["sel2"])
            TT("dve", sel2g, sel2g, keepg[:, :].unsqueeze(2).to_broadcast([128, 128, 8]), ALU.mult, ["sel2", "keepg"], ["sel2"])
            TS("dve", sel2[:], sel2[:], -10.0, ALU.add, ["sel2"], ["sel2"])
            for i in range(16):
                S.op("dve", lambda e: e.max(out=top8[:, i, :], in_=sel2[:, i, :]), ["sel2"], ["top8"])
            TT("dve", sel[:], sel2[:], top8[:, :, 5:6].to_broadcast([128, 16, 64]), ALU.is_ge, ["sel2", "top8"], ["sel"])
            TT("dve", sel[:], sel[:], scores[:], ALU.mult, ["sel", "scores"], ["sel"])
            S.op("dve", lambda e: e.tensor_reduce(out=nrm[:], in_=sel[:], axis=AX.X, op=ALU.add), ["sel"], ["nrm"])
            RCP(nrm[:], nrm[:], ["nrm"], ["nrm"])
            STT(Wr[:], sel[:], 2.5, nrm[:, :].unsqueeze(2).to_broadcast([128, 16, 64]), ALU.mult, ALU.mult, ["sel", "nrm"], ["Wr"])
            dump("Wr", Wr[:], [128, 16, 64], F32, "Wr")
            S.barrier()
        wgs = sb(stM, "wgs", [128, 8, 512], F32)
        wds = sb(stM, "wds", [128, 2, 1024], F32)
        wgb = [sb(stM, "wgb%d" % i, [128, 8, 512], BF16) for i in range(2)]
        wdb = [sb(stM, "wdb%d" % i, [128, 2, 1024], BF16) for i in range(2)]
        sgb = sb(stM, "sgb", [128, 512], BF16)
        actb = sb(stM, "actb", [128, 2, 512], BF16)
        l_e = [S.lane("exg%d" % s), S.lane("exd%d" % s)]
        n_exp = 65 if stage >= 4 else 2
        for e_ in range(n_exp):
            ex = e_ if stage >= 4 else (0 if e_ == 0 else 64)
            b = e_ % 2
            S.dma("sp", l_e[0], [
                lambda e: e.dma_start(out=wgs[:, :, 0:256], in_=wg_d[ex].rearrange("(k p) c -> p k c", p=128)),
                lambda e: e.dma_start(out=wgs[:, :, 256:512], in_=wu_d[ex].rearrange("(k p) c -> p k c", p=128)),
            ], [], ["wgs"])
            DMA("sp", l_e[1], wds[:], wd_d[ex].rearrange("(k p) c -> p k c", p=128), [], ["wds"])
            CP("pool", wgb[b][:], wgs[:], ["wgs"], ["wgb%d" % b])
            CP("pool", wdb[b][:], wds[:], ["wds"], ["wdb%d" % b])
            for t in range(4):
                tsl = slice(t * 512, (t + 1) * 512)
                for c in range(2):
                    for k in range(8):
                        MM(P[1 + c][:, :], wgb[b][:, k, c * 128:(c + 1) * 128], hT[:, k, tsl], k == 0, k == 7, ["wgb%d" % b, "hT"], ["P%d" % (1 + c)])
                    for k in range(8):
                        MM(P[3 + c][:, :], wgb[b][:, k, 256 + c * 128:256 + (c + 1) * 128], hT[:, k, tsl], k == 0, k == 7,
                           ["wgb%d" % b, "hT"], ["P%d" % (3 + c)])
                for c in range(2):
                    ACT(sgb[:], P[1 + c][:, :], AF.Silu, ["P%d" % (1 + c)], ["sgb"])
                    TT("dve", actb[:, c, :], P[3 + c][:, :], sgb[:], ALU.mult, ["P%d" % (3 + c), "sgb"], ["actb"])
                for j in range(4):
                    i = t * 4 + j
                    for half in range(2):
                        pb = P[5 + half]
                        pk = "P%d" % (5 + half)
                        for c in range(2):
                            MM(pb[:, :], actb[:, c, j * 128:(j + 1) * 128], wdb[b][:, c, half * 512:(half + 1) * 512], c == 0, c == 1,
                               ["actb", "wdb%d" % b], [pk])
                        av = acc[:, i, half * 512:(half + 1) * 512]
                        wsc = Wr[:, i, ex:ex + 1] if ex < 64 else 1.0
                        if e_ == 0:
                            TS("dve", av, pb[:, :], wsc, ALU.mult, [pk, "Wr"], ["acc"])
                        else:
                            STT(av, pb[:, :], wsc, av, ALU.mult, ALU.add, [pk, "Wr", "acc"], ["acc"])
        gt2bc = sb(stM, "gt2bc", [128, 1024], F32)
        gfbc = sb(stM, "gfbc", [128, 1024], F32)
        rbt2 = sb(stM, "rbt2", [128, 4, 128], F32)
        xin2 = sb(stM, "xin2", [128, 1024], F32)
        ot = [sb(stM, "ot%d" % i, [128, 1024], F32) for i in range(2)]
        junk2 = sb(stM, "junk2", [128, 1024], BF16)
        st2 = sb(stM, "st2", [128, 4], F32)
        l_x2 = S.lane("x1ld%d" % s)
        l_o = [S.lane("out%d_%d" % (s, i)) for i in range(2)]
        row_bcast(gt2bc, lambda k: modS[:, 40 + k, s:s + 1], rbt2, ["modS"], "gt2bc")
        row_bcast(gfbc, lambda k: gvec[:, 2, k:k + 1], rbt2, ["consts"], "gfbc")
        for i in range(16):
            ts_ = slice(i * 128, (i + 1) * 128)
            b = i % 2
            DMA("sp", l_x2, xin2[:], x1_d[ts_, :], ["x1_d"], ["xin2"])
            TT("dve", acc[:, i, :], acc[:, i, :], gt2bc[:], ALU.mult, ["acc", "gt2bc"], ["acc"])
            TT("pool", acc[:, i, :], acc[:, i, :], xin2[:], ALU.add, ["acc", "xin2"], ["acc"])
            ACT(junk2[:], acc[:, i, :], AF.Square, ["acc"], ["junk2", "st2a"], accum=st2[:, 0:1])
            ACT(st2[:, 1:2], st2[:, 0:1], AF.Sqrt, ["st2a"], ["st2b"], bias=EPS, scale=1.0 / 1024)
            RCP(st2[:, 2:3], st2[:, 1:2], ["st2b"], ["st2c"])
            STT(ot[b][:], acc[:, i, :], st2[:, 2:3], gfbc[:], ALU.mult, ALU.mult, ["acc", "st2c", "gfbc"], ["ot%d" % b])
            DMA("sp", l_o[b], out_d[s, ts_, :], ot[b][:], ["ot%d" % b], [])
        S.barrier()
        stM.close()
    S.barrier()
    st0.close()
    return nc, dump_d


def _masks():
    idx = np.arange(128)
    k = idx[:, None]; i = idx[None, :]
    sc = (k // 64) == (i // 64)
    m = np.zeros((128, 12, 128), np.float32)
    m[:, 0] = sc & (k <= i)
    m[:, 1] = sc & (k >= i)
    m[:, 2] = sc & (k > i)
    m[:, 3] = sc & (k < i)
    m[:, 4] = np.where(sc & (k > i), 0.0, NEG)
    m[:, 5] = np.where(sc & (k < i), 0.0, NEG)
    m[:, 6] = np.where(sc & (i >= k), 0.0, NEG)
    m[:, 7] = np.where(sc & (i <= k), 0.0, NEG)
    m[:, 8] = (k < 64) & (i >= 0)
    m[:, 9] = (k >= 64) & (i >= 0)
    m[:, 10] = (k == i)
    m[:, 11] = 1.0
    negrep = np.stack([np.tile(m[:, 4 + q], (1, HG)) for q in range(4)], axis=1).astype(np.float32)
    return m, np.ascontiguousarray(negrep)


def _prep_inputs(inp, core):
    f = lambda a: np.ascontiguousarray(a, dtype=np.float32)
    b0 = 2 * core
    cT = np.zeros((128, 8, 4), np.float32)
    cT[:, :, 0] = inp["c"][b0].reshape(8, 128).T
    cT[:, :, 1] = inp["c"][b0 + 1].reshape(8, 128).T
    cT[:, :, 2] = inp["c_ctx"].reshape(8, 128).T
    gvec = np.stack([inp["g_mix"][0], inp["g_ffn"][0], inp["g_final"]]).reshape(3, 8, 128).transpose(2, 0, 1)
    r0 = np.zeros(16, np.float32); r1 = np.zeros(16, np.float32)
    for d in range(2):
        for h in range(4):
            r0[d * 8 + 4 + h] = inp["dn_dt_bias"][0, d, h]
            r1[d * 8 + 4 + h] = inp["dn_a_log"][0, d, h]
    bac = np.broadcast_to(np.stack([np.tile(r0, 16), np.tile(r1, 16)])[None], (128, 2, 256))
    masks, negrep = _masks()
    cfv = np.stack([inp["cf_dw_b"][0], inp["cf_ln_g"][0], inp["cf_ln_b"][0]]).reshape(3, 4, 128).transpose(2, 0, 1)
    return {
        "x": f(inp["x"][b0:b0 + 2]), "ctx": f(inp["ctx"][b0:b0 + 2]), "cT": cT,
        "w_mod": f(inp["w_mod"][0]), "b_modT": f(inp["b_mod"][0].reshape(48, 128).T),
        "gvec": f(gvec), "w_in": f(inp["w_in"][0]), "w_out": f(inp["w_out"][0]),
        "convw": f(inp["dn_conv_w"][0].reshape(7, 12, 128).transpose(2, 1, 0)),
        "bac": f(bac), "normg": f(inp["dn_norm_g"][0].reshape(128, 1)),
        "cfw": f(inp["cf_dw_w"][0].reshape(31, 4, 128).transpose(2, 1, 0)), "cfv": f(cfv),
        "router_w": f(inp["router_w"][0]), "rbias": f(np.broadcast_to(inp["router_bias"][0][None], (128, 64))),
        "wg": inp["_wg"], "wu": inp["_wu"], "wd": inp["_wd"],
        "masks": masks, "negrep": negrep,
    }


def kernel(**inputs):
    inp = {k: np.asarray(v) for k, v in inputs.items()}
    inp["_wg"] = np.ascontiguousarray(np.concatenate([inp["exp_w_gate"][0], inp["sh_w_gate"]], axis=0), dtype=np.float32)
    inp["_wu"] = np.ascontiguousarray(np.concatenate([inp["exp_w_up"][0], inp["sh_w_up"]], axis=0), dtype=np.float32)
    inp["_wd"] = np.ascontiguousarray(np.concatenate([inp["exp_w_down"][0], inp["sh_w_down"]], axis=0), dtype=np.float32)
    nc, _ = build_nc()
    in_maps = [_prep_inputs(inp, c) for c in range(8)]
    res = run_bass_kernel_spmd(nc, in_maps, core_ids=list(range(8)))
    out = np.concatenate([r["out"] for r in res.results], axis=0)
    return out.astype(np.float32)
```

```python
from contextlib import ExitStack
import numpy as np
import concourse.bass as bass
import concourse.mybir as mybir
from concourse.bass_utils import run_bass_kernel_spmd

F32 = mybir.dt.float32
BF16 = mybir.dt.bfloat16
I32 = mybir.dt.int32
U32 = mybir.dt.uint32
AF = mybir.ActivationFunctionType
ALU = mybir.AluOpType
AX = mybir.AxisListType


class Lane:
    def __init__(self, sem, name):
        self.sem = sem
        self.count = 0
        self.name = name


class Sched:
    ENGS = ("pe", "act", "dve", "pool", "sp")

    def __init__(self, nc, stack):
        self.nc = nc
        self.stack = stack
        self.ops = {e: [] for e in self.ENGS}
        self.cnt = {e: 0 for e in self.ENGS}
        self.sem = {e: stack.enter_context(nc.semaphore("s_" + e)) for e in self.ENGS}
        self.waited = {e: {} for e in self.ENGS}
        self.last_w = {}
        self.readers = {}
        self.lanes = []
        self.n_wait = 0
        self.n_op = 0
        self.E = {"pe": nc.tensor, "act": nc.scalar, "dve": nc.vector, "pool": nc.gpsimd, "sp": nc.sync}

    def lane(self, name):
        ln = Lane(self.stack.enter_context(self.nc.semaphore("l_" + name)), name)
        self.lanes.append(ln)
        return ln

    def _need(self, eng, prod, val, needs):
        if prod is None:
            return
        cur = needs.get(prod, 0)
        if val > cur:
            needs[prod] = val

    def _deps(self, eng, reads, writes, self_prod):
        needs = {}
        for k in reads:
            lw = self.last_w.get(k)
            if lw is not None:
                self._need(eng, lw[0], lw[1], needs)
        same_ok = (self_prod == "pe") or not isinstance(self_prod, str)
        for k in writes:
            lw = self.last_w.get(k)
            if lw is not None and (lw[0] != self_prod or not same_ok):
                self._need(eng, lw[0], lw[1], needs)
            for p, v in self.readers.get(k, {}).items():
                if p != self_prod or not same_ok:
                    self._need(eng, p, v, needs)
        return needs

    def _emit_waits(self, eng, needs):
        w = self.waited[eng]
        for prod, val in needs.items():
            if w.get(prod, 0) >= val:
                continue
            w[prod] = val
            sem = self.sem[prod] if isinstance(prod, str) else prod.sem
            self.E[eng].wait_ge(sem, val)
            self.n_wait += 1

    def _record(self, prod, val, reads, writes):
        for k in writes:
            self.last_w[k] = (prod, val)
            self.readers[k] = {}
        for k in reads:
            r = self.readers.setdefault(k, {})
            if r.get(prod, 0) < val:
                r[prod] = val

    limit = None

    def op(self, eng, fn, reads=(), writes=()):
        if self.limit is not None and self.n_op >= self.limit:
            return
        needs = self._deps(eng, reads, writes, eng)
        self._emit_waits(eng, needs)
        self.cnt[eng] += 1
        val = self.cnt[eng]
        fn(self.E[eng]).then_inc(self.sem[eng], 1)
        self.n_op += 1
        self._record(eng, val, reads, writes)

    def dma(self, eng, lane, fns, reads=(), writes=()):
        needs = self._deps(eng, reads, writes, lane)
        if lane.count > 0:
            self._need(eng, lane, lane.count, needs)
        self._emit_waits(eng, needs)
        for fn in fns:
            lane.count += 16
            fn(self.E[eng]).then_inc(lane.sem, 16)
        self._record(lane, lane.count, reads, writes)

    def wait_all(self, eng, lanes):
        for ln in lanes:
            if ln.count > 0:
                self._emit_waits(eng, {ln: ln.count})

    def barrier(self):
        for e in self.ENGS:
            needs = {p: self.cnt[p] for p in self.ENGS if p != e and self.cnt[p] > 0}
            for ln in self.lanes:
                if ln.count > 0:
                    needs[ln] = ln.count
            self._emit_waits(e, needs)


HG = 2
NEG = -30000.0
EPS = 1e-6


def build_nc(nseq=2, stage=99, dumps=(), dn_limit=None):
    nc = bass.Bass("TRN2", target_bir_lowering=False)

    def din(name, shape, dt=F32):
        return nc.dram_tensor(name, shape, dt, kind="ExternalInput").ap()

    x_d = din("x", [2, 2048, 1024]); ctx_d = din("ctx", [2, 256, 1024])
    cT_d = din("cT", [128, 8, 4]); wmod_d = din("w_mod", [1024, 6144]); bmod_d = din("b_modT", [128, 48])
    gvec_d = din("gvec", [128, 3, 8]); win_d = din("w_in", [1024, 3088]); wout_d = din("w_out", [1024, 1024])
    convw_d = din("convw", [128, 12, 7]); bac_d = din("bac", [128, 2, 256]); normg_d = din("normg", [128, 1])
    cfw_d = din("cfw", [128, 4, 31]); cfv_d = din("cfv", [128, 3, 4])
    rw_d = din("router_w", [1024, 64]); rb_d = din("rbias", [128, 64])
    wg_d = din("wg", [65, 1024, 256]); wu_d = din("wu", [65, 1024, 256]); wd_d = din("wd", [65, 256, 1024])
    masks_d = din("masks", [128, 12, 128]); negrep_d = din("negrep", [128, 4, HG * 128])
    out_d = nc.dram_tensor("out", [2, 2048, 1024], F32, kind="ExternalOutput").ap()
    x1_d = nc.dram_tensor("x1s", [2048, 1024], F32, kind="Internal").ap()
    hT_d = nc.dram_tensor("hTs", [1024, 2048], BF16, kind="Internal").ap()
    dump_d = {}

    st0 = ExitStack()
    S = Sched(nc, st0)

    uniq = [0]

    def sb(st, name, shape, dt):
        uniq[0] += 1
        return st.enter_context(nc.sbuf_tensor("sb%d_%s" % (uniq[0], name), shape, dt))

    def MM(out, lhsT, rhs, start, stop, r, w):
        S.op("pe", lambda e: e.matmul(out, lhsT=lhsT, rhs=rhs, start=start, stop=stop), r, w)

    def TR(out, in_, ident, r, w):
        S.op("pe", lambda e: e.transpose(out, in_, ident), r, w)

    def ACT(out, in_, func, r, w, bias=None, scale=None, accum=None):
        kw = {}
        if bias is not None:
            kw["bias"] = bias
        if scale is not None:
            kw["scale"] = scale
        if accum is not None:
            kw["accum_out"] = accum
        S.op("act", lambda e: e.activation(out=out, in_=in_, func=func, **kw), r, w)

    def TT(eng, out, in0, in1, op, r, w):
        S.op(eng, lambda e: e.tensor_tensor(out=out, in0=in0, in1=in1, op=op), r, w)

    def TS(eng, out, in0, s1, op0, r, w, s2=None, op1=None):
        if s2 is None:
            S.op(eng, lambda e: e.tensor_scalar(out=out, in0=in0, scalar1=s1, scalar2=None, op0=op0), r, w)
        else:
            S.op(eng, lambda e: e.tensor_scalar(out=out, in0=in0, scalar1=s1, scalar2=s2, op0=op0, op1=op1), r, w)

    def STT(out, in0, scalar, in1, op0, op1, r, w):
        S.op("dve", lambda e: e.scalar_tensor_tensor(out=out, in0=in0, scalar=scalar, in1=in1, op0=op0, op1=op1), r, w)

    def CP(eng, out, in_, r, w):
        if eng == "act":
            S.op("act", lambda e: e.activation(out=out, in_=in_, func=AF.Identity), r, w)
        else:
            S.op(eng, lambda e: e.tensor_copy(out=out, in_=in_), r, w)

    def RCP(out, in_, r, w):
        S.op("dve", lambda e: e.reciprocal(out=out, in_=in_), r, w)

    def MSET(eng, ap, val, w):
        S.op(eng, lambda e: e.memset(ap, val), (), w)

    def DMA(eng, lane, out, in_, r, w):
        S.dma(eng, lane, [lambda e: e.dma_start(out=out, in_=in_)], r, w)

    dump_lane = S.lane("dump")

    def dump(name, ap, shape, dt, key):
        if name not in dumps:
            return
        d = nc.dram_tensor("dbg_" + name, list(shape), dt, kind="ExternalOutput").ap()
        dump_d[name] = d
        DMA("sp", dump_lane, d, ap, [key], [])

    ps_tr = st0.enter_context(nc.psum_tensor("ps_tr", [128, 1024], BF16))
    P = [st0.enter_context(nc.psum_tensor("ps%d" % i, [128, 512], F32)) for i in range(7)]

    masks = sb(st0, "masks", [128, 12, 128], F32)
    negrep = sb(st0, "negrep", [128, 4, HG * 128], F32)
    identb = sb(st0, "identb", [128, 128], BF16)
    onesb = sb(st0, "onesb", [128, 128], BF16)
    modS = sb(st0, "modS", [128, 48, 4], F32)
    G1 = sb(st0, "G1", [128, 8, 4], F32)
    G2 = sb(st0, "G2", [128, 8, 4], F32)
    gvec = sb(st0, "gvec", [128, 3, 8], F32)
    bmod = sb(st0, "bmod", [128, 48], F32)
    rbias = sb(st0, "rbias", [128, 64], F32)
    bac = sb(st0, "bac", [128, 2, 256], F32)
    negA = sb(st0, "negA", [128, 256], F32)
    convw = sb(st0, "convw", [128, 12, 7], F32)
    cfw = sb(st0, "cfw", [128, 4, 31], F32)
    cfv = sb(st0, "cfv", [128, 3, 4], F32)
    normg = sb(st0, "normg", [128, 1], F32)
    rwb = sb(st0, "rwb", [128, 8, 64], BF16)
    identf = masks[:, 10, :]
    onesf = masks[:, 11, :]
    TRI = [masks[:, 0, :], masks[:, 1, :]]
    STR = [masks[:, 2, :], masks[:, 3, :]]
    CH = [masks[:, 8, :], masks[:, 9, :]]

    l_c = S.lane("consts")
    S.dma("sp", l_c, [
        lambda e: e.dma_start(out=masks[:], in_=masks_d),
        lambda e: e.dma_start(out=negrep[:], in_=negrep_d),
        lambda e: e.dma_start(out=gvec[:], in_=gvec_d),
        lambda e: e.dma_start(out=bmod[:], in_=bmod_d),
        lambda e: e.dma_start(out=rbias[:], in_=rb_d),
        lambda e: e.dma_start(out=bac[:], in_=bac_d),
        lambda e: e.dma_start(out=convw[:], in_=convw_d),
        lambda e: e.dma_start(out=cfw[:], in_=cfw_d),
        lambda e: e.dma_start(out=cfv[:], in_=cfv_d),
        lambda e: e.dma_start(out=normg[:], in_=normg_d),
    ], [], ["consts"])
    CP("dve", identb[:], identf, ["consts"], ["identb"])
    CP("dve", onesb[:], onesf, ["consts"], ["onesb"])
    ACT(negA[:], bac[:, 1, :], AF.Exp, ["consts"], ["negA"])
    TS("dve", negA[:], negA[:], -1.0, ALU.mult, ["negA"], ["negA"])

    with ExitStack() as stp:
        scT = sb(stp, "scT", [128, 8, 4], F32)
        wm = [sb(stp, "wm%d" % i, [128, 8, 1024], F32) for i in range(2)]
        rwf = sb(stp, "rwf", [128, 8, 64], F32)
        l_wm = [S.lane("wm0"), S.lane("wm1")]
        DMA("sp", l_c, scT[:], cT_d, [], ["scT"])
        DMA("sp", l_c, rwf[:], rw_d.rearrange("(k p) e -> p k e", p=128), [], ["rwf"])
        ACT(scT[:], scT[:], AF.Silu, ["scT"], ["scT"])
        CP("pool", rwb[:], rwf[:], ["rwf"], ["rwb"])
        wmv = wmod_d.rearrange("(k p) c -> p k c", p=128)
        pm = P[0][:, 0:192].rearrange("p (c j) -> p c j", j=4)
        for cb in range(6):
            b = cb % 2
            DMA("sp", l_wm[b], wm[b][:], wmv[:, :, cb * 1024:(cb + 1) * 1024], [], ["wm%d" % b])
            for j in range(8):
                ch = cb * 8 + j
                for k in range(8):
                    MM(pm[:, ch, :], wm[b][:, k, j * 128:(j + 1) * 128], scT[:, k, :], k == 0, k == 7,
                       ["wm%d" % b, "scT"], ["P0"])
        for j in range(3):
            TT("dve", modS[:, :, j], pm[:, :, j], bmod[:], ALU.add, ["P0", "consts"], ["modS"])
        for j in range(3):
            STT(G1[:, :, j], modS[:, 8:16, j], 1.0, gvec[:, 0, :], ALU.add, ALU.mult, ["modS", "consts"], ["G1"])
            STT(G2[:, :, j], modS[:, 32:40, j], 1.0, gvec[:, 1, :], ALU.add, ALU.mult, ["modS", "consts"], ["G2"])
    S.barrier()
    dump("modS", modS[:], [128, 48, 4], F32, "modS")
    if stage <= 0:
        S.barrier()
        st0.close()
        return nc, dump_d

    def row_bcast(dst, colsrc, tmp, keys_r, key_w, tkey="rb_tmp"):
        for half in range(2):
            for kk in range(4):
                k = half * 4 + kk
                TS("dve", tmp[:, kk, :], identf, colsrc(k), ALU.mult, keys_r + ["consts"], [tkey])
                MM(P[1][:, kk * 128:(kk + 1) * 128], onesf, tmp[:, kk, :], True, True, [tkey, "consts"], ["P1"])
            CP("act", dst[:, half * 512:(half + 1) * 512], P[1][:, :], ["P1"], [key_w])


    for s in range(nseq):
        stA = ExitStack()
        xnT = sb(stA, "xnT", [128, 8, 2048], BF16)
        cfT = sb(stA, "cfT", [128, 4, 2048], BF16)
        dnT = sb(stA, "dnT", [128, 4, 2048], BF16)
        qT = sb(stA, "qT", [128, HG, 2048], BF16)
        kT = sb(stA, "kT", [128, HG, 2048], BF16)
        cvT = sb(stA, "cvT", [128, HG, 2048], BF16)
        zsT = sb(stA, "zsT", [128, HG, 2048], BF16)
        ostore = sb(stA, "ostore", [128, 16, HG, 128], F32)
        ubuf = sb(stA, "ubuf", [128, 2056], BF16)
        ybuf = sb(stA, "ybuf", [128, 3968], BF16)
        wst = [sb(stA, "wst%d" % i, [128, 8, 128], F32) for i in range(2)]
        wcb = [sb(stA, "wcb%d" % i, [128, 8, 128], BF16) for i in range(2)]
        dgq = sb(stA, "dgq", [128, 7, 128], BF16)
        dgc = sb(stA, "dgc", [128, 31, 128], BF16)
        xin = sb(stA, "xin", [128, 1024], F32)
        xs = sb(stA, "xs", [128, 1024], BF16)
        junk = sb(stA, "junk", [128, 1024], BF16)
        st1 = sb(stA, "st1", [128, 8], F32)
        tmpa = sb(stA, "tmpa", [128, 512], F32)
        tmpb = sb(stA, "tmpb", [128, 512], F32)
        tmpc = sb(stA, "tmpc", [128, 512], BF16)
        gall = sb(stA, "gall", [128, 288], F32)
        ball = sb(stA, "ball", [128, 288], F32)
        nball = sb(stA, "nball", [128, 288], F32)
        xnTc = sb(stA, "xnTc", [128, 8, 256], BF16)
        baA = sb(stA, "baA", [128, 256], F32)
        wbab = sb(stA, "wbab", [128, 8, 16], BF16)
        wbaf = sb(stA, "wbaf", [128, 8, 16], F32)
        l_x = S.lane("xin%d" % s)
        l_w = [S.lane("wst%d_%d" % (s, i)) for i in range(2)]
        wctr = [0]

        MSET("pool", ubuf[:], 0.0, ["ubuf"])
        MSET("pool", ybuf[:], 0.0, ["ybuf"])

        def norm_mod_T(src, skey, j, Gm, shbase, dstT, col0, key_dst):
            ACT(junk[:], src, AF.Square, [skey], ["junk", "st1a"], accum=st1[:, 0:1])
            ACT(st1[:, 1:2], st1[:, 0:1], AF.Sqrt, ["st1a"], ["st1b"], bias=EPS, scale=1.0 / 1024)
            RCP(st1[:, 2:3], st1[:, 1:2], ["st1b"], ["st1c"])
            TS("dve", xs[:], src, st1[:, 2:3], ALU.mult, [skey, "st1c"], ["xs"])
            for k in range(8):
                TR(ps_tr[:, k * 128:(k + 1) * 128], xs[:, k * 128:(k + 1) * 128], identb[:], ["xs", "identb"], ["ps_tr"])
            for k in range(8):
                if k % 2 == 0:
                    ACT(dstT[:, k, col0:col0 + 128], ps_tr[:, k * 128:(k + 1) * 128], AF.Identity,
                        ["ps_tr", "G1", "G2", "modS"], [key_dst], bias=modS[:, shbase + k, j:j + 1], scale=Gm[:, k, j:j + 1])
                else:
                    TS("dve", dstT[:, k, col0:col0 + 128], ps_tr[:, k * 128:(k + 1) * 128], Gm[:, k, j:j + 1], ALU.mult,
                       ["ps_tr", "G1", "G2", "modS"], [key_dst], s2=modS[:, shbase + k, j:j + 1], op1=ALU.add)

        def load_w(col0, ncols=128):
            b = wctr[0] % 2
            wctr[0] += 1
            DMA("sp", l_w[b], wst[b][:, :, 0:ncols], win_d.rearrange("(k p) c -> p k c", p=128)[:, :, col0:col0 + ncols],
                [], ["wst%d" % b])
            CP("pool", wcb[b][:, :, 0:ncols], wst[b][:, :, 0:ncols], ["wst%d" % b], ["wcb%d" % b])
            return wcb[b], "wcb%d" % b

        def proj_tile(wt, wkey, xsrc, xkey, t0, tw, pbank, pkey):
            for k in range(8):
                MM(pbank[:, 0:tw], wt[:, k, :], xsrc[:, k, t0:t0 + tw], k == 0, k == 7, [wkey, xkey], [pkey])

        def l2norm_store(src_bf, dst, scale, bias):
            tw = src_bf.shape[1]
            TT("dve", tmpc[:, 0:tw], src_bf, src_bf, ALU.mult, ["cs"], ["tmpc"])
            MM(P[3][:, 0:tw], onesb[:], tmpc[:, 0:tw], True, True, ["tmpc", "onesb"], ["P3"])
            ACT(tmpa[:, 0:tw], P[3][:, 0:tw], AF.Sqrt, ["P3"], ["tmpa"], bias=bias, scale=scale)
            RCP(tmpb[:, 0:tw], tmpa[:, 0:tw], ["tmpa"], ["tmpb"])
            TT("dve", dst, src_bf, tmpb[:, 0:tw], ALU.mult, ["cs", "tmpb"], ["qkv"])

        def conv_chunk(cc, N, kind, dst, xsrc, xkey):
            wt, wkey = load_w(cc * 128)
            tw = min(512, N)
            for t in range(N // tw):
                proj_tile(wt, wkey, xsrc, xkey, t * tw, tw, P[1], "P1")
                CP("dve", ubuf[:, 4 + t * tw:4 + (t + 1) * tw], P[1][:, 0:tw], ["P1"], ["ubuf"])
            if N < 2048:
                MSET("pool", ubuf[:, 4 + N:8 + N], 0.0, ["ubuf"])
            for jt in range(7):
                TS("dve", dgq[:, jt, :], identb[:], convw[:, cc, jt:jt + 1], ALU.mult, ["identb", "consts"], ["dgq"])
            cs = sb_cs
            for t in range(N // tw):
                for jt in range(7):
                    MM(P[2][:, 0:tw], dgq[:, jt, :], ubuf[:, t * tw + jt + 1:t * tw + jt + 1 + tw], jt == 0, jt == 6, ["dgq", "ubuf"], ["P2"])
                if kind == "v":
                    ACT(dst[:, t * tw:(t + 1) * tw], P[2][:, 0:tw], AF.Silu, ["P2"], ["qkv"])
                else:
                    ACT(cs[:, 0:tw], P[2][:, 0:tw], AF.Silu, ["P2"], ["cs"])
                    if kind == "q":
                        l2norm_store(cs[:, 0:tw], dst[:, t * tw:(t + 1) * tw], 128.0, 128.0 * EPS)
                    else:
                        l2norm_store(cs[:, 0:tw], dst[:, t * tw:(t + 1) * tw], 1.0, EPS)

        sb_cs = sb(stA, "cs", [128, 512], BF16)

        def ba_all(N, xsrc, xkey, tb):
            nt = N // 128
            DMA("sp", l_x, wbaf[:], win_d.rearrange("(k p) c -> p k c", p=128)[:, :, 2048:2064], [], ["wbaf"])
            CP("pool", wbab[:], wbaf[:], ["wbaf"], ["wbab"])
            for i in range(nt):
                for k in range(8):
                    MM(P[1][:, i * 16:(i + 1) * 16], xsrc[:, k, i * 128:(i + 1) * 128], wbab[:, k, :], k == 0, k == 7,
                       [xkey, "wbab"], ["P1"])
            w = nt * 16
            o = tb * 16
            TT("dve", baA[:, 0:w], P[1][:, 0:w], bac[:, 0, 0:w], ALU.add, ["P1", "consts"], ["baA"])
            ACT(baA[:, 0:w], baA[:, 0:w], AF.Exp, ["baA"], ["baA"])
            ACT(baA[:, 0:w], baA[:, 0:w], AF.Ln, ["baA"], ["baA"], bias=1.0)
            TT("dve", gall[:, o:o + w], baA[:, 0:w], negA[:, 0:w], ALU.mult, ["baA", "negA"], ["gall"])
            ACT(ball[:, o:o + w], P[1][:, 0:w], AF.Sigmoid, ["P1"], ["ball"])
            TS("dve", nball[:, o:o + w], ball[:, o:o + w], -1.0, ALU.mult, ["ball"], ["nball"])

        exs2 = [sb(stA, "exs%d" % i, [128, 16], F32) for i in range(2)]
        bg = sb(stA, "bg", [128, HG], F32)
        gtri = sb(stA, "gtri", [128, HG * 128], F32)
        decs = sb(stA, "decs", [128, HG * 128], BF16)
        dect = sb(stA, "dect", [128, HG * 128], BF16)
        Pb = sb(stA, "Pb", [128, HG, 128], BF16)
        ATb2 = [sb(stA, "ATb%d" % i, [128, HG, 128], BF16) for i in range(2)]
        Qb = sb(stA, "Qb", [128, HG, 128], BF16)
        Tb = sb(stA, "Tb", [128, HG, 128], BF16)
        kdb2 = [sb(stA, "kdb%d" % i, [128, HG, 128], BF16) for i in range(2)]
        kbgb = sb(stA, "kbgb", [128, HG, 128], BF16)
        vbb = sb(stA, "vbb", [128, HG, 128], BF16)
        usb2 = [sb(stA, "usb%d" % i, [128, HG * 128], F32) for i in range(2)]
        wTb2 = [sb(stA, "wTb%d" % i, [128, HG, 128], BF16) for i in range(2)]
        vn = sb(stA, "vn", [128, HG, 128], BF16)
        osb = sb(stA, "osb", [128, HG * 128], F32)
        otmp = sb(stA, "otmp", [128, HG * 128], F32)
        Sb = [[sb(stA, "S%d_%d" % (d, h), [128, 128], BF16) for h in range(HG)] for d in range(2)]
        ont = sb(stA, "ont", [128, HG, 128], BF16)
        ms = sb(stA, "ms", [128, 8], F32)
        MSET("pool", vn[:], 0.0, ["vn"])

        def dn_prep(g, i, d, lat, pb):
            exs, ATb, kdb, wTb, usb = exs2[pb], ATb2[pb], kdb2[pb], wTb2[pb], usb2[pb]
            kx, ka, kk_, kw, ku = "exs%d" % pb, "ATb%d" % pb, "kdb%d" % pb, "wTb%d" % pb, "usb%d" % pb
            tc0 = i * 128
            ib = i if lat else 16 + i
            gcol = ib * 16 + d * 8 + 4 + g * HG
            bcol = ib * 16 + d * 8 + g * HG
            g4 = ib * 16 + d * 8 + 4
            MM(P[2][:, 256:260], TRI[d], gall[:, g4:g4 + 4], True, True, ["consts", "gall"], ["P2"])
            MM(P[2][:, 260:264], CH[0], gall[:, g4:g4 + 4], True, True, ["consts", "gall"], ["P2"])
            MM(P[2][:, 264:268], CH[1], gall[:, g4:g4 + 4], True, True, ["consts", "gall"], ["P2"])
            MM(P[2][:, 268:272], STR[d], gall[:, g4:g4 + 4], True, True, ["consts", "gall"], ["P2"])
            ACT(exs[:, 0:16], P[2][:, 256:272], AF.Exp, ["P2"], [kx])
            yield
            e0 = g * HG
            TT("dve", bg[:], exs[:, e0:e0 + 2], ball[:, bcol:bcol + 2], ALU.mult, [kx, "ball"], ["bg"])
            for h in range(HG):
                TS("pool", gtri[:, h * 128:(h + 1) * 128], TRI[d], gall[:, gcol + h:gcol + h + 1], ALU.mult,
                   ["consts", "gall"], ["gtri"])
            MM(P[1][:, 0:HG * 128], identf, negrep[:, d, :], True, False, ["consts"], ["P1"])
            for h in range(HG):
                MM(P[1][:, h * 128:(h + 1) * 128], gtri[:, h * 128:(h + 1) * 128], STR[d], False, h == HG - 1, ["gtri", "consts"], ["P1"])
            MM(P[2][:, 0:HG * 128], identf, negrep[:, 2 + d, :], True, False, ["consts"], ["P2"])
            MM(P[2][:, 0:HG * 128], STR[d], gtri[:], False, True, ["gtri", "consts"], ["P2"])
            ACT(decs[:], P[1][:, 0:HG * 128], AF.Exp, ["P1"], ["decs"])
            ACT(dect[:], P[2][:, 0:HG * 128], AF.Exp, ["P2"], ["dect"])
            yield
            for h in range(HG):
                MM(P[3][:, h * 128:(h + 1) * 128], kT[:, h, tc0:tc0 + 128], kT[:, h, tc0:tc0 + 128], True, True, ["qkv"], ["P3"])
            if lat:
                for h in range(HG):
                    MM(P[3][:, 256 + h * 128:256 + (h + 1) * 128], kT[:, h, tc0:tc0 + 128], qT[:, h, tc0:tc0 + 128], True, True, ["qkv"], ["P3"])
            for h in range(HG):
                STT(Pb[:, h, :], P[3][:, h * 128:(h + 1) * 128], nball[:, bcol + h:bcol + h + 1], decs[:, h * 128:(h + 1) * 128],
                    ALU.mult, ALU.mult, ["P3", "nball", "decs"], ["Pb"])
            if lat:
                TT("dve", ATb[:].rearrange("p h c -> p (h c)"), P[3][:, 256:512], dect[:], ALU.mult, ["P3", "dect"], [ka])
            yield
            for h in range(HG):
                TR(ps_tr[:, h * 128:(h + 1) * 128], kT[:, h, tc0:tc0 + 128], identb[:], ["qkv", "identb"], ["ps_tr"])
                TR(ps_tr[:, 256 + h * 128:256 + (h + 1) * 128], cvT[:, h, tc0:tc0 + 128], identb[:], ["qkv", "identb"], ["ps_tr"])
                TR(ps_tr[:, 512 + h * 128:512 + (h + 1) * 128], Pb[:, h, :], identb[:], ["Pb", "identb"], ["ps_tr"])
            for h in range(HG):
                ACT(kdb[:, h, :], ps_tr[:, h * 128:(h + 1) * 128], AF.Identity, ["ps_tr", kx], [kk_], scale=exs[:, 12 + e0 + h:13 + e0 + h])
                ACT(kbgb[:, h, :], ps_tr[:, h * 128:(h + 1) * 128], AF.Identity, ["ps_tr", "bg"], ["kbgb"], scale=bg[:, h:h + 1])
                ACT(vbb[:, h, :], ps_tr[:, 256 + h * 128:256 + (h + 1) * 128], AF.Identity, ["ps_tr", "ball"], ["vbb"],
                    scale=ball[:, bcol + h:bcol + h + 1])
                CP("act", Qb[:, h, :], ps_tr[:, 512 + h * 128:512 + (h + 1) * 128], ["ps_tr"], ["QTq"])
                CP("pool", Tb[:, h, :], identb[:], ["identb"], ["QTt"])
            yield
            p4 = P[4][:, :].rearrange("p (h t c) -> p h t c", h=HG, t=2)
            p5 = P[5][:, 0:HG * 128].rearrange("p (h c) -> p h c", h=HG)
            for m in range(5):
                for h in range(HG):
                    MM(P[4][:, h * 256:h * 256 + 128], Pb[:, h, :], Qb[:, h, :], True, True, ["Pb", "QTq"], ["P4"])
                    MM(P[4][:, h * 256 + 128:(h + 1) * 256], Pb[:, h, :], Tb[:, h, :], True, True, ["Pb", "QTt"], ["P4"])
                for h in range(HG):
                    MM(P[5][:, h * 128:(h + 1) * 128], Qb[:, h, :], Pb[:, h, :], True, True, ["Pb", "QTq"], ["P5"])
                TT("dve", Tb[:], Tb[:], p4[:, :, 1, :], ALU.add, ["QTt", "P4"], ["QTt"])
                for h in range(HG):
                    CP("dve", Qb[:, h, :], P[4][:, h * 256:h * 256 + 128], ["P4"], ["QTq"])
                CP("dve", Pb[:].rearrange("p h c -> p (h c)"), P[5][:, 0:HG * 128], ["P5"], ["Pb"])
                yield
            for h in range(HG):
                MM(P[5][:, h * 128:(h + 1) * 128], Pb[:, h, :], Tb[:, h, :], True, True, ["Pb", "QTt"], ["P5"])
            TT("dve", Tb[:], Tb[:], p5, ALU.add, ["QTt", "P5"], ["QTt"])
            for h in range(HG):
                MM(P[3][:, h * 128:(h + 1) * 128], Tb[:, h, :], vbb[:, h, :], True, True, ["QTt", "vbb"], ["P3"])
                MM(P[5][:, h * 128:(h + 1) * 128], kbgb[:, h, :], Tb[:, h, :], True, True, ["QTt", "kbgb"], ["P5"])
            CP("dve", usb[:], P[3][:, 0:HG * 128], ["P3"], [ku])
            CP("dve", wTb[:].rearrange("p h c -> p (h c)"), P[5][:, 0:HG * 128], ["P5"], [kw])

        def dn_scan(g, i, d, lat, pb):
            tc0 = i * 128
            e0 = g * HG
            exs, ATb, kdb, wTb, usb = exs2[pb], ATb2[pb], kdb2[pb], wTb2[pb], usb2[pb]
            kx, ka, kk_, kw, ku = "exs%d" % pb, "ATb%d" % pb, "kdb%d" % pb, "wTb%d" % pb, "usb%d" % pb
            for c in ([0, 1] if d == 0 else [1, 0]):
                pr = slice(c * 64, (c + 1) * 64)
                for h in range(HG):
                    MM(P[6][pr, h * 128:(h + 1) * 128], wTb[:, h, pr], Sb[d][h][:], True, True, [kw, "S%d%d" % (d, h)], ["P6"])
                if lat:
                    for h in range(HG):
                        MM(P[6][pr, 256 + h * 128:256 + (h + 1) * 128], qT[:, h, tc0 + c * 64:tc0 + (c + 1) * 64], Sb[d][h][:], True, True,
                           ["qkv", "S%d%d" % (d, h)], ["P6"])
                TT("dve", vn[pr].rearrange("p h c -> p (h c)"), usb[pr, :], P[6][pr, 0:HG * 128], ALU.subtract, [ku, "P6"], ["vn"])
                yield
                if lat:
                    for h in range(HG):
                        TS("dve", osb[pr, h * 128:(h + 1) * 128], P[6][pr, 256 + h * 128:256 + (h + 1) * 128], exs[pr, e0 + h:e0 + h + 1],
                           ALU.mult, ["P6", kx], ["osb"])
                    for h in range(HG):
                        MM(P[0][pr, h * 128:(h + 1) * 128], ATb[pr, h, pr], vn[pr, h, :], True, True, [ka, "vn"], ["P0"])
                for h in range(HG):
                    MM(P[0][:, 256 + h * 128:256 + (h + 1) * 128], kdb[pr, h, :], vn[pr, h, :], True, True, [kk_, "vn"], ["P0"])
                if lat:
                    oview = ostore[pr, i, :, :].rearrange("p h c -> p (h c)")
                    if d == 0:
                        TT("dve", oview, osb[pr, :], P[0][pr, 0:256], ALU.add, ["osb", "P0"], ["ostore"])
                    else:
                        TT("dve", otmp[pr, :], osb[pr, :], P[0][pr, 0:256], ALU.add, ["osb", "P0"], ["otmp"])
                        TT("pool", oview, oview, otmp[pr, :], ALU.add, ["otmp", "ostore"], ["ostore"])
                for h in range(HG):
                    STT(Sb[d][h][:], Sb[d][h][:], exs[:, 4 + 4 * c + e0 + h:5 + 4 * c + e0 + h], P[0][:, 256 + h * 128:256 + (h + 1) * 128],
                        ALU.mult, ALU.add, ["S%d%d" % (d, h), kx, "P0"], ["S%d%d" % (d, h)])
                yield


        def dn_run(tiles):
            def drain(gen):
                for _ in gen:
                    pass
            prev = None
            for n, (g, i, d, lat) in enumerate(tiles):
                pg = dn_prep(g, i, d, lat, n % 2)
                if prev is None:
                    drain(pg)
                else:
                    sg = dn_scan(*prev)
                    alive_p, alive_s = True, True
                    while alive_p or alive_s:
                        if alive_p:
                            alive_p = next(pg, "end") != "end"
                        if alive_s:
                            alive_s = next(sg, "end") != "end"
                prev = (g, i, d, lat, n % 2)
            drain(dn_scan(*prev))

        tmpd = sb(stA, "tmpd", [128, 512], F32)

        def conformer():
            for cc in range(4):
                wa, wak = load_w(2064 + cc * 128)
                wgt, wgk = load_w(2576 + cc * 128)
                for t in range(4):
                    proj_tile(wa, wak, xnT, "xnT", t * 512, 512, P[1], "P1")
                    proj_tile(wgt, wgk, xnT, "xnT", t * 512, 512, P[2], "P2")
                    ACT(tmpa[:], P[2][:, :], AF.Sigmoid, ["P2"], ["tmpa"])
                    TT("dve", ybuf[:, 960 + t * 512:960 + (t + 1) * 512], P[1][:, :], tmpa[:], ALU.mult, ["P1", "tmpa"], ["ybuf"])
                for jt in range(31):
                    TS("dve", dgc[:, jt, :], identb[:], cfw[:, cc, jt:jt + 1], ALU.mult, ["identb", "consts"], ["dgc"])
                for t in range(4):
                    for jt in range(31):
                        MM(P[1][:, :], dgc[:, jt, :], ybuf[:, t * 512 + jt * 64:t * 512 + jt * 64 + 512], jt == 0, jt == 30,
                           ["dgc", "ybuf"], ["P1"])
                    ACT(cfT[:, cc, t * 512:(t + 1) * 512], P[1][:, :], AF.Identity, ["P1", "consts"], ["cfT"], bias=cfv[:, 0, cc:cc + 1])
            for t in range(4):
                sl = slice(t * 512, (t + 1) * 512)
                for cc in range(4):
                    MM(P[5][:, :], onesb[:], cfT[:, cc, sl], cc == 0, cc == 3, ["onesb", "cfT"], ["P5"])
                for cc in range(4):
                    TT("dve", tmpc[:], cfT[:, cc, sl], cfT[:, cc, sl], ALU.mult, ["cfT"], ["tmpc"])
                    MM(P[6][:, :], onesb[:], tmpc[:], cc == 0, cc == 3, ["onesb", "tmpc"], ["P6"])
                TS("dve", tmpa[:], P[5][:, :], 1.0 / 512, ALU.mult, ["P5"], ["tmpa"])
                TT("dve", tmpb[:], tmpa[:], tmpa[:], ALU.mult, ["tmpa"], ["tmpb"])
                STT(tmpb[:], P[6][:, :], 1.0 / 512, tmpb[:], ALU.mult, ALU.subtract, ["P6", "tmpb"], ["tmpb"])
                ACT(tmpb[:], tmpb[:], AF.Sqrt, ["tmpb"], ["tmpb"], bias=EPS)
                RCP(tmpb[:], tmpb[:], ["tmpb"], ["tmpb"])
                for cc in range(4):
                    TT("dve", tmpd[:], cfT[:, cc, sl], tmpa[:], ALU.subtract, ["cfT", "tmpa"], ["tmpd"])
                    TT("dve", tmpd[:], tmpd[:], tmpb[:], ALU.mult, ["tmpd", "tmpb"], ["tmpd"])
                    ACT(cfT[:, cc, sl], tmpd[:], AF.Silu, ["tmpd", "consts"], ["cfT"], bias=cfv[:, 2, cc:cc + 1], scale=cfv[:, 1, cc:cc + 1])

        def dn_finish(g):
            for i in range(16):
                for h in range(HG):
                    ACT(junk[:, 0:128], ostore[:, i, h, :], AF.Square, ["ostore"], ["junk", "ms_a"], accum=ms[:, h:h + 1])
                ACT(ms[:, 2:4], ms[:, 0:2], AF.Sqrt, ["ms_a"], ["ms_b"], bias=EPS, scale=1.0 / 128)
                RCP(ms[:, 4:6], ms[:, 2:4], ["ms_b"], ["ms_c"])
                for h in range(HG):
                    TS("dve", ont[:, h, :], ostore[:, i, h, :], ms[:, 4 + h:5 + h], ALU.mult, ["ostore", "ms_c"], ["ont"])
                for h in range(HG):
                    TR(ps_tr[:, 768 + h * 128:768 + (h + 1) * 128], ont[:, h, :], identb[:], ["ont", "identb"], ["ps_tr"])
                for h in range(HG):
                    STT(dnT[:, g * HG + h, i * 128:(i + 1) * 128], ps_tr[:, 768 + h * 128:768 + (h + 1) * 128], normg[:, 0:1],
                        zsT[:, h, i * 128:(i + 1) * 128], ALU.mult, ALU.mult, ["ps_tr", "consts", "zsT"], ["dnT"])

        for i in range(2):
            DMA("sp", l_x, xin[:], ctx_d[s, i * 128:(i + 1) * 128, :], [], ["xin"])
            norm_mod_T(xin[:], "xin", 2, G1, 0, xnTc, i * 128, "xnTc")
        for i in range(16):
            DMA("sp", l_x, xin[:], x_d[s, i * 128:(i + 1) * 128, :], [], ["xin"])
            norm_mod_T(xin[:], "xin", s, G1, 0, xnT, i * 128, "xnT")
        dump("xnT", xnT[:], [128, 8, 2048], BF16, "xnT")
        if stage == 0.5:
            S.barrier(); stA.close(); continue
        ba_all(256, xnTc, "xnTc", 16)
        ba_all(2048, xnT, "xnT", 0)
        dump("gall", gall[:], [128, 288], F32, "gall")
        dump("ball", ball[:], [128, 288], F32, "ball")
        if stage == 0.7:
            S.barrier(); stA.close(); continue
        if stage >= 2:
            conformer()
            dump("cfT", cfT[:], [128, 4, 2048], BF16, "cfT")
        for g in range(4 // HG):
            for d in range(2):
                for h in range(HG):
                    MSET("pool", Sb[d][h][:], 0.0, ["S%d%d" % (d, h)])
            for h in range(HG):
                conv_chunk(4 + g * HG + h, 256, "k", kT[:, h, 0:256], xnTc, "xnTc")
                conv_chunk(8 + g * HG + h, 256, "v", cvT[:, h, 0:256], xnTc, "xnTc")
            dn_run([(g, 0, 0, False), (g, 1, 0, False), (g, 1, 1, False), (g, 0, 1, False)])
            if g == 0:
                dump("kTc", kT[:, :, 0:256], [128, HG, 256], BF16, "qkv")
                dump("Sctx", Sb[0][0][:], [128, 128], BF16, "S00")
            for h in range(HG):
                hh = g * HG + h
                wz, wzk = load_w(1536 + hh * 128)
                for t in range(4):
                    proj_tile(wz, wzk, xnT, "xnT", t * 512, 512, P[1], "P1")
                    ACT(zsT[:, h, t * 512:(t + 1) * 512], P[1][:, :], AF.Silu, ["P1"], ["zsT"])
                conv_chunk(hh, 2048, "q", qT[:, h, :], xnT, "xnT")
                conv_chunk(4 + hh, 2048, "k", kT[:, h, :], xnT, "xnT")
                conv_chunk(8 + hh, 2048, "v", cvT[:, h, :], xnT, "xnT")
            if g == 0:
                dump("qT", qT[:], [128, HG, 2048], BF16, "qkv")
                dump("kT", kT[:], [128, HG, 2048], BF16, "qkv")
                dump("cvT", cvT[:], [128, HG, 2048], BF16, "qkv")
            dn_run([(g, i, 0, True) for i in range(16)] + [(g, i, 1, True) for i in range(15, -1, -1)])
            if g == 0:
                dump("ostore", ostore[:], [128, 16, HG, 128], F32, "ostore")
            dn_finish(g)
        dump("dnT", dnT[:], [128, 4, 2048], BF16, "dnT")
        if stage <= 2:
            S.barrier()
            stA.close()
            continue
        wo_h = [qT[:].rearrange("p a (b c) -> p (a b) c", c=512), kT[:].rearrange("p a (b c) -> p (a b) c", c=512)]
        gt1bc = sb(stA, "gt1bc", [128, 1024], F32)
        rbt = tmpd[:].rearrange("p (a c) -> p a c", c=128)
        xhalf = [tmpa, tmpb]
        hTt1 = sb(stA, "hTt", [128, 8, 128], BF16)
        hTt = [hTt1, hTt1]
        l_x1 = S.lane("x1st%d" % s)
        l_h = [S.lane("hst%d_%d" % (s, i)) for i in range(2)]
        for c8 in range(8):
            b = wctr[0] % 2
            wctr[0] += 1
            DMA("sp", l_w[b], wst[b][:], wout_d.rearrange("(k p) c -> p k c", p=128)[:, :, c8 * 128:(c8 + 1) * 128], [], ["wst%d" % b])
            CP("pool", wo_h[c8 // 4][:, :, (c8 % 4) * 128:(c8 % 4 + 1) * 128], wst[b][:], ["wst%d" % b], ["qkv"])
        row_bcast(gt1bc, lambda k: modS[:, 16 + k, s:s + 1], rbt, ["modS", "tmpd"], "gt1bc", "tmpd")
        hTv = hT_d.rearrange("(k p) t -> p k t", p=128)
        for i in range(16):
            ts_ = slice(i * 128, (i + 1) * 128)
            DMA("sp", l_x, xin[:], x_d[s, ts_, :], [], ["xin"])
            for half in range(2):
                for k in range(8):
                    src = dnT[:, k, ts_] if k < 4 else cfT[:, k - 4, ts_]
                    MM(P[1 + half][:, :], src, wo_h[half][:, k, :], k == 0, k == 7,
                       ["dnT", "cfT", "qkv"], ["P%d" % (1 + half)])
                hk = "tmpa" if half == 0 else "tmpb"
                TT("dve", xhalf[half][:], P[1 + half][:, :], gt1bc[:, half * 512:(half + 1) * 512], ALU.mult,
                   ["P%d" % (1 + half), "gt1bc"], [hk])
                TT("pool", xin[:, half * 512:(half + 1) * 512], xin[:, half * 512:(half + 1) * 512], xhalf[half][:], ALU.add,
                   [hk, "xin"], ["xin"])
            DMA("sp", l_x1, x1_d[ts_, :], xin[:], ["xin"], ["x1_d"])
            b = i % 2
            norm_mod_T(xin[:], "xin", s, G2, 24, hTt[b], 0, "hTt")
            DMA("sp", l_h[b], hTv[:, :, ts_], hTt[b][:], ["hTt"], ["hT_d"])
        S.barrier()
        stA.close()

        stM = ExitStack()
        hT = sb(stM, "hT", [128, 8, 2048], BF16)
        acc = sb(stM, "acc", [128, 16, 1024], F32)
        Wr = sb(stM, "Wr", [128, 16, 64], F32)
        l_m = S.lane("moe_in%d" % s)
        DMA("sp", l_m, hT[:], hTv, ["hT_d"], ["hT"])
        with ExitStack() as stR:
            scores = sb(stR, "scores", [128, 16, 64], F32)
            sel = sb(stR, "sel", [128, 16, 64], F32)
            sel2 = sb(stR, "sel2", [128, 16, 64], F32)
            m1 = sb(stR, "m1", [128, 128], F32)
            m2 = sb(stR, "m2", [128, 128], F32)
            top8 = sb(stR, "top8", [128, 16, 8], F32)
            keepg = sb(stR, "keepg", [128, 128], F32)
            nrm = sb(stR, "nrm", [128, 16], F32)
            for i in range(16):
                for k in range(8):
                    MM(P[0][:, (i % 8) * 64:(i % 8 + 1) * 64], hT[:, k, i * 128:(i + 1) * 128], rwb[:, k, :], k == 0, k == 7, ["hT", "rwb"], ["P0"])
                if i % 8 == 7:
                    ACT(scores[:, i - 7:i + 1, :].rearrange("p a e -> p (a e)"), P[0][:, :], AF.Sigmoid, ["P0"], ["scores"])
            TT("dve", sel[:], scores[:], rbias[:, :].unsqueeze(1).to_broadcast([128, 16, 64]), ALU.add, ["scores", "consts"], ["sel"])
            selg = sel[:].rearrange("p a (g e) -> p (a g) e", e=8)
            sel2g = sel2[:].rearrange("p a (g e) -> p (a g) e", e=8)
            S.op("dve", lambda e: e.tensor_reduce(out=m1[:], in_=selg, axis=AX.X, op=ALU.max), ["sel"], ["m1"])
            TT("dve", sel2g, selg, m1[:, :].unsqueeze(2).to_broadcast([128, 128, 8]), ALU.is_equal, ["sel", "m1"], ["sel2"])
            STT(sel2[:], sel2[:], -1e9, sel[:], ALU.mult, ALU.add, ["sel2", "sel"], ["sel2"])
            S.op("dve", lambda e: e.tensor_reduce(out=m2[:], in_=sel2g, axis=AX.X, op=ALU.max), ["sel2"], ["m2"])
            TT("dve", m1[:], m1[:], m2[:], ALU.add, ["m1", "m2"], ["m1"])
            for i in range(16):
                S.op("dve", lambda e: e.max(out=top8[:, i, :], in_=m1[:, i * 8:(i + 1) * 8]), ["m1"], ["top8"])
            TT("dve", keepg[:].rearrange("p (a g) -> p a g", g=8), m1[:].rearrange("p (a g) -> p a g", g=8),
               top8[:, :, 3:4].to_broadcast([128, 16, 8]), ALU.is_ge, ["m1", "top8"], ["keepg"])
            TS("dve", sel2[:], sel[:], 10.0, ALU.add, ["sel"], ["sel2"])
            TT("dve", sel2g, sel2g, keepg[:, :].unsqueeze(2).to_broadcast([128, 128, 8]), ALU.mult, ["sel2", "keepg"], ["sel2"])
            TS("dve", sel2[:], sel2[:], -10.0, ALU.add, ["sel2"], ["sel2"])
            for i in range(16):
                S.op("dve", lambda e: e.max(out=top8[:, i, :], in_=sel2[:, i, :]), ["sel2"], ["top8"])
            TT("dve", sel[:], sel2[:], top8[:, :, 5:6].to_broadcast([128, 16, 64]), ALU.is_ge, ["sel2", "top8"], ["sel"])
            TT("dve", sel[:], sel[:], scores[:], ALU.mult, ["sel", "scores"], ["sel"])
            S.op("dve", lambda e: e.tensor_reduce(out=nrm[:], in_=sel[:], axis=AX.X, op=ALU.add), ["sel"], ["nrm"])
            RCP(nrm[:], nrm[:], ["nrm"], ["nrm"])
            STT(Wr[:], sel[:], 2.5, nrm[:, :].unsqueeze(2).to_broadcast([128, 16, 64]), ALU.mult, ALU.mult, ["sel", "nrm"], ["Wr"])
            dump("Wr", Wr[:], [128, 16, 64], F32, "Wr")
            S.barrier()
        wgs = sb(stM, "wgs", [128, 8, 512], F32)
        wds = sb(stM, "wds", [128, 2, 1024], F32)
        wgb = [sb(stM, "wgb%d" % i, [128, 8, 512], BF16) for i in range(2)]
        wdb = [sb(stM, "wdb%d" % i, [128, 2, 1024], BF16) for i in range(2)]
        sgb = sb(stM, "sgb", [128, 512], BF16)
        actb = sb(stM, "actb", [128, 2, 512], BF16)
        l_e = [S.lane("exg%d" % s), S.lane("exd%d" % s)]
        n_exp = 65 if stage >= 4 else 2
        for e_ in range(n_exp):
            ex = e_ if stage >= 4 else (0 if e_ == 0 else 64)
            b = e_ % 2
            S.dma("sp", l_e[0], [
                lambda e: e.dma_start(out=wgs[:, :, 0:256], in_=wg_d[ex].rearrange("(k p) c -> p k c", p=128)),
                lambda e: e.dma_start(out=wgs[:, :, 256:512], in_=wu_d[ex].rearrange("(k p) c -> p k c", p=128)),
            ], [], ["wgs"])
            DMA("sp", l_e[1], wds[:], wd_d[ex].rearrange("(k p) c -> p k c", p=128), [], ["wds"])
            CP("pool", wgb[b][:], wgs[:], ["wgs"], ["wgb%d" % b])
            CP("pool", wdb[b][:], wds[:], ["wds"], ["wdb%d" % b])
            for t in range(4):
                tsl = slice(t * 512, (t + 1) * 512)
                for c in range(2):
                    for k in range(8):
                        MM(P[1 + c][:, :], wgb[b][:, k, c * 128:(c + 1) * 128], hT[:, k, tsl], k == 0, k == 7, ["wgb%d" % b, "hT"], ["P%d" % (1 + c)])
                    for k in range(8):
                        MM(P[3 + c][:, :], wgb[b][:, k, 256 + c * 128:256 + (c + 1) * 128], hT[:, k, tsl], k == 0, k == 7,
                           ["wgb%d" % b, "hT"], ["P%d" % (3 + c)])
                for c in range(2):
                    ACT(sgb[:], P[1 + c][:, :], AF.Silu, ["P%d" % (1 + c)], ["sgb"])
                    TT("dve", actb[:, c, :], P[3 + c][:, :], sgb[:], ALU.mult, ["P%d" % (3 + c), "sgb"], ["actb"])
                for j in range(4):
                    i = t * 4 + j
                    for half in range(2):
                        pb = P[5 + half]
                        pk = "P%d" % (5 + half)
                        for c in range(2):
                            MM(pb[:, :], actb[:, c, j * 128:(j + 1) * 128], wdb[b][:, c, half * 512:(half + 1) * 512], c == 0, c == 1,
                               ["actb", "wdb%d" % b], [pk])
                        av = acc[:, i, half * 512:(half + 1) * 512]
                        wsc = Wr[:, i, ex:ex + 1] if ex < 64 else 1.0
                        if e_ == 0:
                            TS("dve", av, pb[:, :], wsc, ALU.mult, [pk, "Wr"], ["acc"])
                        else:
                            STT(av, pb[:, :], wsc, av, ALU.mult, ALU.add, [pk, "Wr", "acc"], ["acc"])
        gt2bc = sb(stM, "gt2bc", [128, 1024], F32)
        gfbc = sb(stM, "gfbc", [128, 1024], F32)
        rbt2 = sb(stM, "rbt2", [128, 4, 128], F32)
        xin2 = sb(stM, "xin2", [128, 1024], F32)
        ot = [sb(stM, "ot%d" % i, [128, 1024], F32) for i in range(2)]
        junk2 = sb(stM, "junk2", [128, 1024], BF16)
        st2 = sb(stM, "st2", [128, 4], F32)
        l_x2 = S.lane("x1ld%d" % s)
        l_o = [S.lane("out%d_%d" % (s, i)) for i in range(2)]
        row_bcast(gt2bc, lambda k: modS[:, 40 + k, s:s + 1], rbt2, ["modS"], "gt2bc")
        row_bcast(gfbc, lambda k: gvec[:, 2, k:k + 1], rbt2, ["consts"], "gfbc")
        for i in range(16):
            ts_ = slice(i * 128, (i + 1) * 128)
            b = i % 2
            DMA("sp", l_x2, xin2[:], x1_d[ts_, :], ["x1_d"], ["xin2"])
            TT("dve", acc[:, i, :], acc[:, i, :], gt2bc[:], ALU.mult, ["acc", "gt2bc"], ["acc"])
            TT("pool", acc[:, i, :], acc[:, i, :], xin2[:], ALU.add, ["acc", "xin2"], ["acc"])
            ACT(junk2[:], acc[:, i, :], AF.Square, ["acc"], ["junk2", "st2a"], accum=st2[:, 0:1])
            ACT(st2[:, 1:2], st2[:, 0:1], AF.Sqrt, ["st2a"], ["st2b"], bias=EPS, scale=1.0 / 1024)
            RCP(st2[:, 2:3], st2[:, 1:2], ["st2b"], ["st2c"])
            STT(ot[b][:], acc[:, i, :], st2[:, 2:3], gfbc[:], ALU.mult, ALU.mult, ["acc", "st2c", "gfbc"], ["ot%d" % b])
            DMA("sp", l_o[b], out_d[s, ts_, :], ot[b][:], ["ot%d" % b], [])
        S.barrier()
        stM.close()
    S.barrier()
    st0.close()
    return nc, dump_d


def _masks():
    idx = np.arange(128)
    k = idx[:, None]; i = idx[None, :]
    sc = (k // 64) == (i // 64)
    m = np.zeros((128, 12, 128), np.float32)
    m[:, 0] = sc & (k <= i)
    m[:, 1] = sc & (k >= i)
    m[:, 2] = sc & (k > i)
    m[:, 3] = sc & (k < i)
    m[:, 4] = np.where(sc & (k > i), 0.0, NEG)
    m[:, 5] = np.where(sc & (k < i), 0.0, NEG)
    m[:, 6] = np.where(sc & (i >= k), 0.0, NEG)
    m[:, 7] = np.where(sc & (i <= k), 0.0, NEG)
    m[:, 8] = (k < 64) & (i >= 0)
    m[:, 9] = (k >= 64) & (i >= 0)
    m[:, 10] = (k == i)
    m[:, 11] = 1.0
    negrep = np.stack([np.tile(m[:, 4 + q], (1, HG)) for q in range(4)], axis=1).astype(np.float32)
    return m, np.ascontiguousarray(negrep)


def _prep_inputs(inp, core):
    f = lambda a: np.ascontiguousarray(a, dtype=np.float32)
    b0 = 2 * core
    cT = np.zeros((128, 8, 4), np.float32)
    cT[:, :, 0] = inp["c"][b0].reshape(8, 128).T
    cT[:, :, 1] = inp["c"][b0 + 1].reshape(8, 128).T
    cT[:, :, 2] = inp["c_ctx"].reshape(8, 128).T
    gvec = np.stack([inp["g_mix"][0], inp["g_ffn"][0], inp["g_final"]]).reshape(3, 8, 128).transpose(2, 0, 1)
    r0 = np.zeros(16, np.float32); r1 = np.zeros(16, np.float32)
    for d in range(2):
        for h in range(4):
            r0[d * 8 + 4 + h] = inp["dn_dt_bias"][0, d, h]
            r1[d * 8 + 4 + h] = inp["dn_a_log"][0, d, h]
    bac = np.broadcast_to(np.stack([np.tile(r0, 16), np.tile(r1, 16)])[None], (128, 2, 256))
    masks, negrep = _masks()
    cfv = np.stack([inp["cf_dw_b"][0], inp["cf_ln_g"][0], inp["cf_ln_b"][0]]).reshape(3, 4, 128).transpose(2, 0, 1)
    return {
        "x": f(inp["x"][b0:b0 + 2]), "ctx": f(inp["ctx"][b0:b0 + 2]), "cT": cT,
        "w_mod": f(inp["w_mod"][0]), "b_modT": f(inp["b_mod"][0].reshape(48, 128).T),
        "gvec": f(gvec), "w_in": f(inp["w_in"][0]), "w_out": f(inp["w_out"][0]),
        "convw": f(inp["dn_conv_w"][0].reshape(7, 12, 128).transpose(2, 1, 0)),
        "bac": f(bac), "normg": f(inp["dn_norm_g"][0].reshape(128, 1)),
        "cfw": f(inp["cf_dw_w"][0].reshape(31, 4, 128).transpose(2, 1, 0)), "cfv": f(cfv),
        "router_w": f(inp["router_w"][0]), "rbias": f(np.broadcast_to(inp["router_bias"][0][None], (128, 64))),
        "wg": inp["_wg"], "wu": inp["_wu"], "wd": inp["_wd"],
        "masks": masks, "negrep": negrep,
    }


def kernel(**inputs):
    inp = {k: np.asarray(v) for k, v in inputs.items()}
    inp["_wg"] = np.ascontiguousarray(np.concatenate([inp["exp_w_gate"][0], inp["sh_w_gate"]], axis=0), dtype=np.float32)
    inp["_wu"] = np.ascontiguousarray(np.concatenate([inp["exp_w_up"][0], inp["sh_w_up"]], axis=0), dtype=np.float32)
    inp["_wd"] = np.ascontiguousarray(np.concatenate([inp["exp_w_down"][0], inp["sh_w_down"]], axis=0), dtype=np.float32)
    nc, _ = build_nc()
    in_maps = [_prep_inputs(inp, c) for c in range(8)]
    res = run_bass_kernel_spmd(nc, in_maps, core_ids=list(range(8)))
    out = np.concatenate([r["out"] for r in res.results], axis=0)
    return out.astype(np.float32)
```

```python
from contextlib import ExitStack
import numpy as np
import concourse.bass as bass
import concourse.mybir as mybir
from concourse.bass_utils import run_bass_kernel_spmd

F32 = mybir.dt.float32
BF16 = mybir.dt.bfloat16
I32 = mybir.dt.int32
U32 = mybir.dt.uint32
AF = mybir.ActivationFunctionType
ALU = mybir.AluOpType
AX = mybir.AxisListType


class Lane:
    def __init__(self, sem, name):
        self.sem = sem
        self.count = 0
        self.name = name


class Sched:
    ENGS = ("pe", "act", "dve", "pool", "sp")

    def __init__(self, nc, stack):
        self.nc = nc
        self.stack = stack
        self.ops = {e: [] for e in self.ENGS}
        self.cnt = {e: 0 for e in self.ENGS}
        self.sem = {e: stack.enter_context(nc.semaphore("s_" + e)) for e in self.ENGS}
        self.waited = {e: {} for e in self.ENGS}
        self.last_w = {}
        self.readers = {}
        self.lanes = []
        self.n_wait = 0
        self.n_op = 0
        self.E = {"pe": nc.tensor, "act": nc.scalar, "dve": nc.vector, "pool": nc.gpsimd, "sp": nc.sync}

    def lane(self, name):
        ln = Lane(self.stack.enter_context(self.nc.semaphore("l_" + name)), name)
        self.lanes.append(ln)
        return ln

    def _need(self, eng, prod, val, needs):
        if prod is None:
            return
        cur = needs.get(prod, 0)
        if val > cur:
            needs[prod] = val

    def _deps(self, eng, reads, writes, self_prod):
        needs = {}
        for k in reads:
            lw = self.last_w.get(k)
            if lw is not None:
                self._need(eng, lw[0], lw[1], needs)
        same_ok = (self_prod == "pe") or not isinstance(self_prod, str)
        for k in writes:
            lw = self.last_w.get(k)
            if lw is not None and (lw[0] != self_prod or not same_ok):
                self._need(eng, lw[0], lw[1], needs)
            for p, v in self.readers.get(k, {}).items():
                if p != self_prod or not same_ok:
                    self._need(eng, p, v, needs)
        return needs

    def _emit_waits(self, eng, needs):
        w = self.waited[eng]
        for prod, val in needs.items():
            if w.get(prod, 0) >= val:
                continue
            w[prod] = val
            sem = self.sem[prod] if isinstance(prod, str) else prod.sem
            self.E[eng].wait_ge(sem, val)
            self.n_wait += 1

    def _record(self, prod, val, reads, writes):
        for k in writes:
            self.last_w[k] = (prod, val)
            self.readers[k] = {}
        for k in reads:
            r = self.readers.setdefault(k, {})
            if r.get(prod, 0) < val:
                r[prod] = val

    limit = None

    def op(self, eng, fn, reads=(), writes=()):
        if self.limit is not None and self.n_op >= self.limit:
            return
        needs = self._deps(eng, reads, writes, eng)
        self._emit_waits(eng, needs)
        self.cnt[eng] += 1
        val = self.cnt[eng]
        fn(self.E[eng]).then_inc(self.sem[eng], 1)
        self.n_op += 1
        self._record(eng, val, reads, writes)

    def dma(self, eng, lane, fns, reads=(), writes=()):
        needs = self._deps(eng, reads, writes, lane)
        if lane.count > 0:
            self._need(eng, lane, lane.count, needs)
        self._emit_waits(eng, needs)
        for fn in fns:
            lane.count += 16
            fn(self.E[eng]).then_inc(lane.sem, 16)
        self._record(lane, lane.count, reads, writes)

    def wait_all(self, eng, lanes):
        for ln in lanes:
            if ln.count > 0:
                self._emit_waits(eng, {ln: ln.count})

    def barrier(self):
        for e in self.ENGS:
            needs = {p: self.cnt[p] for p in self.ENGS if p != e and self.cnt[p] > 0}
            for ln in self.lanes:
                if ln.count > 0:
                    needs[ln] = ln.count
            self._emit_waits(e, needs)


HG = 2
NEG = -30000.0
EPS = 1e-6


def build_nc(nseq=2, stage=99, dumps=(), dn_limit=None):
    nc = bass.Bass("TRN2", target_bir_lowering=False)

    def din(name, shape, dt=F32):
        return nc.dram_tensor(name, shape, dt, kind="ExternalInput").ap()

    x_d = din("x", [2, 2048, 1024]); ctx_d = din("ctx", [2, 256, 1024])
    cT_d = din("cT", [128, 8, 4]); wmod_d = din("w_mod", [1024, 6144]); bmod_d = din("b_modT", [128, 48])
    gvec_d = din("gvec", [128, 3, 8]); win_d = din("w_in", [1024, 3088]); wout_d = din("w_out", [1024, 1024])
    convw_d = din("convw", [128, 12, 7]); bac_d = din("bac", [128, 2, 256]); normg_d = din("normg", [128, 1])
    cfw_d = din("cfw", [128, 4, 31]); cfv_d = din("cfv", [128, 3, 4])
    rw_d = din("router_w", [1024, 64]); rb_d = din("rbias", [128, 64])
    wg_d = din("wg", [65, 1024, 256]); wu_d = din("wu", [65, 1024, 256]); wd_d = din("wd", [65, 256, 1024])
    masks_d = din("masks", [128, 12, 128]); negrep_d = din("negrep", [128, 4, HG * 128])
    out_d = nc.dram_tensor("out", [2, 2048, 1024], F32, kind="ExternalOutput").ap()
    x1_d = nc.dram_tensor("x1s", [2048, 1024], F32, kind="Internal").ap()
    hT_d = nc.dram_tensor("hTs", [1024, 2048], BF16, kind="Internal").ap()
    dump_d = {}

    st0 = ExitStack()
    S = Sched(nc, st0)

    uniq = [0]

    def sb(st, name, shape, dt):
        uniq[0] += 1
        return st.enter_context(nc.sbuf_tensor("sb%d_%s" % (uniq[0], name), shape, dt))

    def MM(out, lhsT, rhs, start, stop, r, w):
        S.op("pe", lambda e: e.matmul(out, lhsT=lhsT, rhs=rhs, start=start, stop=stop), r, w)

    def TR(out, in_, ident, r, w):
        S.op("pe", lambda e: e.transpose(out, in_, ident), r, w)

    def ACT(out, in_, func, r, w, bias=None, scale=None, accum=None):
        kw = {}
        if bias is not None:
            kw["bias"] = bias
        if scale is not None:
            kw["scale"] = scale
        if accum is not None:
            kw["accum_out"] = accum
        S.op("act", lambda e: e.activation(out=out, in_=in_, func=func, **kw), r, w)

    def TT(eng, out, in0, in1, op, r, w):
        S.op(eng, lambda e: e.tensor_tensor(out=out, in0=in0, in1=in1, op=op), r, w)

    def TS(eng, out, in0, s1, op0, r, w, s2=None, op1=None):
        if s2 is None:
            S.op(eng, lambda e: e.tensor_scalar(out=out, in0=in0, scalar1=s1, scalar2=None, op0=op0), r, w)
        else:
            S.op(eng, lambda e: e.tensor_scalar(out=out, in0=in0, scalar1=s1, scalar2=s2, op0=op0, op1=op1), r, w)

    def STT(out, in0, scalar, in1, op0, op1, r, w):
        S.op("dve", lambda e: e.scalar_tensor_tensor(out=out, in0=in0, scalar=scalar, in1=in1, op0=op0, op1=op1), r, w)

    def CP(eng, out, in_, r, w):
        if eng == "act":
            S.op("act", lambda e: e.activation(out=out, in_=in_, func=AF.Identity), r, w)
        else:
            S.op(eng, lambda e: e.tensor_copy(out=out, in_=in_), r, w)

    def RCP(out, in_, r, w):
        S.op("dve", lambda e: e.reciprocal(out=out, in_=in_), r, w)

    def MSET(eng, ap, val, w):
        S.op(eng, lambda e: e.memset(ap, val), (), w)

    def DMA(eng, lane, out, in_, r, w):
        S.dma(eng, lane, [lambda e: e.dma_start(out=out, in_=in_)], r, w)

    dump_lane = S.lane("dump")

    def dump(name, ap, shape, dt, key):
        if name not in dumps:
            return
        d = nc.dram_tensor("dbg_" + name, list(shape), dt, kind="ExternalOutput").ap()
        dump_d[name] = d
        DMA("sp", dump_lane, d, ap, [key], [])

    ps_tr = st0.enter_context(nc.psum_tensor("ps_tr", [128, 1024], BF16))
    P = [st0.enter_context(nc.psum_tensor("ps%d" % i, [128, 512], F32)) for i in range(7)]

    masks = sb(st0, "masks", [128, 12, 128], F32)
    negrep = sb(st0, "negrep", [128, 4, HG * 128], F32)
    identb = sb(st0, "identb", [128, 128], BF16)
    onesb = sb(st0, "onesb", [128, 128], BF16)
    modS = sb(st0, "modS", [128, 48, 4], F32)
    G1 = sb(st0, "G1", [128, 8, 4], F32)
    G2 = sb(st0, "G2", [128, 8, 4], F32)
    gvec = sb(st0, "gvec", [128, 3, 8], F32)
    bmod = sb(st0, "bmod", [128, 48], F32)
    rbias = sb(st0, "rbias", [128, 64], F32)
    bac = sb(st0, "bac", [128, 2, 256], F32)
    negA = sb(st0, "negA", [128, 256], F32)
    convw = sb(st0, "convw", [128, 12, 7], F32)
    cfw = sb(st0, "cfw", [128, 4, 31], F32)
    cfv = sb(st0, "cfv", [128, 3, 4], F32)
    normg = sb(st0, "normg", [128, 1], F32)
    rwb = sb(st0, "rwb", [128, 8, 64], BF16)
    identf = masks[:, 10, :]
    onesf = masks[:, 11, :]
    TRI = [masks[:, 0, :], masks[:, 1, :]]
    STR = [masks[:, 2, :], masks[:, 3, :]]
    CH = [masks[:, 8, :], masks[:, 9, :]]

    l_c = S.lane("consts")
    S.dma("sp", l_c, [
        lambda e: e.dma_start(out=masks[:], in_=masks_d),
        lambda e: e.dma_start(out=negrep[:], in_=negrep_d),
        lambda e: e.dma_start(out=gvec[:], in_=gvec_d),
        lambda e: e.dma_start(out=bmod[:], in_=bmod_d),
        lambda e: e.dma_start(out=rbias[:], in_=rb_d),
        lambda e: e.dma_start(out=bac[:], in_=bac_d),
        lambda e: e.dma_start(out=convw[:], in_=convw_d),
        lambda e: e.dma_start(out=cfw[:], in_=cfw_d),
        lambda e: e.dma_start(out=cfv[:], in_=cfv_d),
        lambda e: e.dma_start(out=normg[:], in_=normg_d),
    ], [], ["consts"])
    CP("dve", identb[:], identf, ["consts"], ["identb"])
    CP("dve", onesb[:], onesf, ["consts"], ["onesb"])
    ACT(negA[:], bac[:, 1, :], AF.Exp, ["consts"], ["negA"])
    TS("dve", negA[:], negA[:], -1.0, ALU.mult, ["negA"], ["negA"])

    with ExitStack() as stp:
        scT = sb(stp, "scT", [128, 8, 4], F32)
        wm = [sb(stp, "wm%d" % i, [128, 8, 1024], F32) for i in range(2)]
        rwf = sb(stp, "rwf", [128, 8, 64], F32)
        l_wm = [S.lane("wm0"), S.lane("wm1")]
        DMA("sp", l_c, scT[:], cT_d, [], ["scT"])
        DMA("sp", l_c, rwf[:], rw_d.rearrange("(k p) e -> p k e", p=128), [], ["rwf"])
        ACT(scT[:], scT[:], AF.Silu, ["scT"], ["scT"])
        CP("pool", rwb[:], rwf[:], ["rwf"], ["rwb"])
        wmv = wmod_d.rearrange("(k p) c -> p k c", p=128)
        pm = P[0][:, 0:192].rearrange("p (c j) -> p c j", j=4)
        for cb in range(6):
            b = cb % 2
            DMA("sp", l_wm[b], wm[b][:], wmv[:, :, cb * 1024:(cb + 1) * 1024], [], ["wm%d" % b])
            for j in range(8):
                ch = cb * 8 + j
                for k in range(8):
                    MM(pm[:, ch, :], wm[b][:, k, j * 128:(j + 1) * 128], scT[:, k, :], k == 0, k == 7,
                       ["wm%d" % b, "scT"], ["P0"])
        for j in range(3):
            TT("dve", modS[:, :, j], pm[:, :, j], bmod[:], ALU.add, ["P0", "consts"], ["modS"])
        for j in range(3):
            STT(G1[:, :, j], modS[:, 8:16, j], 1.0, gvec[:, 0, :], ALU.add, ALU.mult, ["modS", "consts"], ["G1"])
            STT(G2[:, :, j], modS[:, 32:40, j], 1.0, gvec[:, 1, :], ALU.add, ALU.mult, ["modS", "consts"], ["G2"])
    S.barrier()
    dump("modS", modS[:], [128, 48, 4], F32, "modS")
    if stage <= 0:
        S.barrier()
        st0.close()
        return nc, dump_d

    def row_bcast(dst, colsrc, tmp, keys_r, key_w, tkey="rb_tmp"):
        for half in range(2):
            for kk in range(4):
                k = half * 4 + kk
                TS("dve", tmp[:, kk, :], identf, colsrc(k), ALU.mult, keys_r + ["consts"], [tkey])
                MM(P[1][:, kk * 128:(kk + 1) * 128], onesf, tmp[:, kk, :], True, True, [tkey, "consts"], ["P1"])
            CP("act", dst[:, half * 512:(half + 1) * 512], P[1][:, :], ["P1"], [key_w])


    for s in range(nseq):
        stA = ExitStack()
        xnT = sb(stA, "xnT", [128, 8, 2048], BF16)
        cfT = sb(stA, "cfT", [128, 4, 2048], BF16)
        dnT = sb(stA, "dnT", [128, 4, 2048], BF16)
        qT = sb(stA, "qT", [128, HG, 2048], BF16)
        kT = sb(stA, "kT", [128, HG, 2048], BF16)
        cvT = sb(stA, "cvT", [128, HG, 2048], BF16)
        zsT = sb(stA, "zsT", [128, HG, 2048], BF16)
        ostore = sb(stA, "ostore", [128, 16, HG, 128], F32)
        ubuf = sb(stA, "ubuf", [128, 2056], BF16)
        ybuf = sb(stA, "ybuf", [128, 3968], BF16)
        wst = [sb(stA, "wst%d" % i, [128, 8, 128], F32) for i in range(2)]
        wcb = [sb(stA, "wcb%d" % i, [128, 8, 128], BF16) for i in range(2)]
        dgq = sb(stA, "dgq", [128, 7, 128], BF16)
        dgc = sb(stA, "dgc", [128, 31, 128], BF16)
        xin = sb(stA, "xin", [128, 1024], F32)
        xs = sb(stA, "xs", [128, 1024], BF16)
        junk = sb(stA, "junk", [128, 1024], BF16)
        st1 = sb(stA, "st1", [128, 8], F32)
        tmpa = sb(stA, "tmpa", [128, 512], F32)
        tmpb = sb(stA, "tmpb", [128, 512], F32)
        tmpc = sb(stA, "tmpc", [128, 512], BF16)
        gall = sb(stA, "gall", [128, 288], F32)
        ball = sb(stA, "ball", [128, 288], F32)
        nball = sb(stA, "nball", [128, 288], F32)
        xnTc = sb(stA, "xnTc", [128, 8, 256], BF16)
        baA = sb(stA, "baA", [128, 256], F32)
        wbab = sb(stA, "wbab", [128, 8, 16], BF16)
        wbaf = sb(stA, "wbaf", [128, 8, 16], F32)
        l_x = S.lane("xin%d" % s)
        l_w = [S.lane("wst%d_%d" % (s, i)) for i in range(2)]
        wctr = [0]

        MSET("pool", ubuf[:], 0.0, ["ubuf"])
        MSET("pool", ybuf[:], 0.0, ["ybuf"])

        def norm_mod_T(src, skey, j, Gm, shbase, dstT, col0, key_dst):
            ACT(junk[:], src, AF.Square, [skey], ["junk", "st1a"], accum=st1[:, 0:1])
            ACT(st1[:, 1:2], st1[:, 0:1], AF.Sqrt, ["st1a"], ["st1b"], bias=EPS, scale=1.0 / 1024)
            RCP(st1[:, 2:3], st1[:, 1:2], ["st1b"], ["st1c"])
            TS("dve", xs[:], src, st1[:, 2:3], ALU.mult, [skey, "st1c"], ["xs"])
            for k in range(8):
                TR(ps_tr[:, k * 128:(k + 1) * 128], xs[:, k * 128:(k + 1) * 128], identb[:], ["xs", "identb"], ["ps_tr"])
            for k in range(8):
                if k % 2 == 0:
                    ACT(dstT[:, k, col0:col0 + 128], ps_tr[:, k * 128:(k + 1) * 128], AF.Identity,
                        ["ps_tr", "G1", "G2", "modS"], [key_dst], bias=modS[:, shbase + k, j:j + 1], scale=Gm[:, k, j:j + 1])
                else:
                    TS("dve", dstT[:, k, col0:col0 + 128], ps_tr[:, k * 128:(k + 1) * 128], Gm[:, k, j:j + 1], ALU.mult,
                       ["ps_tr", "G1", "G2", "modS"], [key_dst], s2=modS[:, shbase + k, j:j + 1], op1=ALU.add)

        def load_w(col0, ncols=128):
            b = wctr[0] % 2
            wctr[0] += 1
            DMA("sp", l_w[b], wst[b][:, :, 0:ncols], win_d.rearrange("(k p) c -> p k c", p=128)[:, :, col0:col0 + ncols],
                [], ["wst%d" % b])
            CP("pool", wcb[b][:, :, 0:ncols], wst[b][:, :, 0:ncols], ["wst%d" % b], ["wcb%d" % b])
            return wcb[b], "wcb%d" % b

        def proj_tile(wt, wkey, xsrc, xkey, t0, tw, pbank, pkey):
            for k in range(8):
                MM(pbank[:, 0:tw], wt[:, k, :], xsrc[:, k, t0:t0 + tw], k == 0, k == 7, [wkey, xkey], [pkey])

        def l2norm_store(src_bf, dst, scale, bias):
            tw = src_bf.shape[1]
            TT("dve", tmpc[:, 0:tw], src_bf, src_bf, ALU.mult, ["cs"], ["tmpc"])
            MM(P[3][:, 0:tw], onesb[:], tmpc[:, 0:tw], True, True, ["tmpc", "onesb"], ["P3"])
            ACT(tmpa[:, 0:tw], P[3][:, 0:tw], AF.Sqrt, ["P3"], ["tmpa"], bias=bias, scale=scale)
            RCP(tmpb[:, 0:tw], tmpa[:, 0:tw], ["tmpa"], ["tmpb"])
            TT("dve", dst, src_bf, tmpb[:, 0:tw], ALU.mult, ["cs", "tmpb"], ["qkv"])

        def conv_chunk(cc, N, kind, dst, xsrc, xkey):
            wt, wkey = load_w(cc * 128)
            tw = min(512, N)
            for t in range(N // tw):
                proj_tile(wt, wkey, xsrc, xkey, t * tw, tw, P[1], "P1")
                CP("dve", ubuf[:, 4 + t * tw:4 + (t + 1) * tw], P[1][:, 0:tw], ["P1"], ["ubuf"])
            if N < 2048:
                MSET("pool", ubuf[:, 4 + N:8 + N], 0.0, ["ubuf"])
            for jt in range(7):
                TS("dve", dgq[:, jt, :], identb[:], convw[:, cc, jt:jt + 1], ALU.mult, ["identb", "consts"], ["dgq"])
            cs = sb_cs
            for t in range(N // tw):
                for jt in range(7):
                    MM(P[2][:, 0:tw], dgq[:, jt, :], ubuf[:, t * tw + jt + 1:t * tw + jt + 1 + tw], jt == 0, jt == 6, ["dgq", "ubuf"], ["P2"])
                if kind == "v":
                    ACT(dst[:, t * tw:(t + 1) * tw], P[2][:, 0:tw], AF.Silu, ["P2"], ["qkv"])
                else:
                    ACT(cs[:, 0:tw], P[2][:, 0:tw], AF.Silu, ["P2"], ["cs"])
                    if kind == "q":
                        l2norm_store(cs[:, 0:tw], dst[:, t * tw:(t + 1) * tw], 128.0, 128.0 * EPS)
                    else:
                        l2norm_store(cs[:, 0:tw], dst[:, t * tw:(t + 1) * tw], 1.0, EPS)

        sb_cs = sb(stA, "cs", [128, 512], BF16)

        def ba_all(N, xsrc, xkey, tb):
            nt = N // 128
            DMA("sp", l_x, wbaf[:], win_d.rearrange("(k p) c -> p k c", p=128)[:, :, 2048:2064], [], ["wbaf"])
            CP("pool", wbab[:], wbaf[:], ["wbaf"], ["wbab"])
            for i in range(nt):
                for k in range(8):
                    MM(P[1][:, i * 16:(i + 1) * 16], xsrc[:, k, i * 128:(i + 1) * 128], wbab[:, k, :], k == 0, k == 7,
                       [xkey, "wbab"], ["P1"])
            w = nt * 16
            o = tb * 16
            TT("dve", baA[:, 0:w], P[1][:, 0:w], bac[:, 0, 0:w], ALU.add, ["P1", "consts"], ["baA"])
            ACT(baA[:, 0:w], baA[:, 0:w], AF.Exp, ["baA"], ["baA"])
            ACT(baA[:, 0:w], baA[:, 0:w], AF.Ln, ["baA"], ["baA"], bias=1.0)
            TT("dve", gall[:, o:o + w], baA[:, 0:w], negA[:, 0:w], ALU.mult, ["baA", "negA"], ["gall"])
            ACT(ball[:, o:o + w], P[1][:, 0:w], AF.Sigmoid, ["P1"], ["ball"])
            TS("dve", nball[:, o:o + w], ball[:, o:o + w], -1.0, ALU.mult, ["ball"], ["nball"])

        exs2 = [sb(stA, "exs%d" % i, [128, 16], F32) for i in range(2)]
        bg = sb(stA, "bg", [128, HG], F32)
        gtri = sb(stA, "gtri", [128, HG * 128], F32)
        decs = sb(stA, "decs", [128, HG * 128], BF16)
        dect = sb(stA, "dect", [128, HG * 128], BF16)
        Pb = sb(stA, "Pb", [128, HG, 128], BF16)
        ATb2 = [sb(stA, "ATb%d" % i, [128, HG, 128], BF16) for i in range(2)]
        Qb = sb(stA, "Qb", [128, HG, 128], BF16)
        Tb = sb(stA, "Tb", [128, HG, 128], BF16)
        kdb2 = [sb(stA, "kdb%d" % i, [128, HG, 128], BF16) for i in range(2)]
        kbgb = sb(stA, "kbgb", [128, HG, 128], BF16)
        vbb = sb(stA, "vbb", [128, HG, 128], BF16)
        usb2 = [sb(stA, "usb%d" % i, [128, HG * 128], F32) for i in range(2)]
        wTb2 = [sb(stA, "wTb%d" % i, [128, HG, 128], BF16) for i in range(2)]
        vn = sb(stA, "vn", [128, HG, 128], BF16)
        osb = sb(stA, "osb", [128, HG * 128], F32)
        otmp = sb(stA, "otmp", [128, HG * 128], F32)
        Sb = [[sb(stA, "S%d_%d" % (d, h), [128, 128], BF16) for h in range(HG)] for d in range(2)]
        ont = sb(stA, "ont", [128, HG, 128], BF16)
        ms = sb(stA, "ms", [128, 8], F32)
        MSET("pool", vn[:], 0.0, ["vn"])

        def dn_prep(g, i, d, lat, pb):
            exs, ATb, kdb, wTb, usb = exs2[pb], ATb2[pb], kdb2[pb], wTb2[pb], usb2[pb]
            kx, ka, kk_, kw, ku = "exs%d" % pb, "ATb%d" % pb, "kdb%d" % pb, "wTb%d" % pb, "usb%d" % pb
            tc0 = i * 128
            ib = i if lat else 16 + i
            gcol = ib * 16 + d * 8 + 4 + g * HG
            bcol = ib * 16 + d * 8 + g * HG
            g4 = ib * 16 + d * 8 + 4
            MM(P[2][:, 256:260], TRI[d], gall[:, g4:g4 + 4], True, True, ["consts", "gall"], ["P2"])
            MM(P[2][:, 260:264], CH[0], gall[:, g4:g4 + 4], True, True, ["consts", "gall"], ["P2"])
            MM(P[2][:, 264:268], CH[1], gall[:, g4:g4 + 4], True, True, ["consts", "gall"], ["P2"])
            MM(P[2][:, 268:272], STR[d], gall[:, g4:g4 + 4], True, True, ["consts", "gall"], ["P2"])
            ACT(exs[:, 0:16], P[2][:, 256:272], AF.Exp, ["P2"], [kx])
            yield
            e0 = g * HG
            TT("dve", bg[:], exs[:, e0:e0 + 2], ball[:, bcol:bcol + 2], ALU.mult, [kx, "ball"], ["bg"])
            for h in range(HG):
                TS("pool", gtri[:, h * 128:(h + 1) * 128], TRI[d], gall[:, gcol + h:gcol + h + 1], ALU.mult,
                   ["consts", "gall"], ["gtri"])
            MM(P[1][:, 0:HG * 128], identf, negrep[:, d, :], True, False, ["consts"], ["P1"])
            for h in range(HG):
                MM(P[1][:, h * 128:(h + 1) * 128], gtri[:, h * 128:(h + 1) * 128], STR[d], False, h == HG - 1, ["gtri", "consts"], ["P1"])
            MM(P[2][:, 0:HG * 128], identf, negrep[:, 2 + d, :], True, False, ["consts"], ["P2"])
            MM(P[2][:, 0:HG * 128], STR[d], gtri[:], False, True, ["gtri", "consts"], ["P2"])
            ACT(decs[:], P[1][:, 0:HG * 128], AF.Exp, ["P1"], ["decs"])
            ACT(dect[:], P[2][:, 0:HG * 128], AF.Exp, ["P2"], ["dect"])
            yield
            for h in range(HG):
                MM(P[3][:, h * 128:(h + 1) * 128], kT[:, h, tc0:tc0 + 128], kT[:, h, tc0:tc0 + 128], True, True, ["qkv"], ["P3"])
            if lat:
                for h in range(HG):
                    MM(P[3][:, 256 + h * 128:256 + (h + 1) * 128], kT[:, h, tc0:tc0 + 128], qT[:, h, tc0:tc0 + 128], True, True, ["qkv"], ["P3"])
            for h in range(HG):
                STT(Pb[:, h, :], P[3][:, h * 128:(h + 1) * 128], nball[:, bcol + h:bcol + h + 1], decs[:, h * 128:(h + 1) * 128],
                    ALU.mult, ALU.mult, ["P3", "nball", "decs"], ["Pb"])
            if lat:
                TT("dve", ATb[:].rearrange("p h c -> p (h c)"), P[3][:, 256:512], dect[:], ALU.mult, ["P3", "dect"], [ka])
            yield
            for h in range(HG):
                TR(ps_tr[:, h * 128:(h + 1) * 128], kT[:, h, tc0:tc0 + 128], identb[:], ["qkv", "identb"], ["ps_tr"])
                TR(ps_tr[:, 256 + h * 128:256 + (h + 1) * 128], cvT[:, h, tc0:tc0 + 128], identb[:], ["qkv", "identb"], ["ps_tr"])
                TR(ps_tr[:, 512 + h * 128:512 + (h + 1) * 128], Pb[:, h, :], identb[:], ["Pb", "identb"], ["ps_tr"])
            for h in range(HG):
                ACT(kdb[:, h, :], ps_tr[:, h * 128:(h + 1) * 128], AF.Identity, ["ps_tr", kx], [kk_], scale=exs[:, 12 + e0 + h:13 + e0 + h])
                ACT(kbgb[:, h, :], ps_tr[:, h * 128:(h + 1) * 128], AF.Identity, ["ps_tr", "bg"], ["kbgb"], scale=bg[:, h:h + 1])
                ACT(vbb[:, h, :], ps_tr[:, 256 + h * 128:256 + (h + 1) * 128], AF.Identity, ["ps_tr", "ball"], ["vbb"],
                    scale=ball[:, bcol + h:bcol + h + 1])
                CP("act", Qb[:, h, :], ps_tr[:, 512 + h * 128:512 + (h + 1) * 128], ["ps_tr"], ["QTq"])
                CP("pool", Tb[:, h, :], identb[:], ["identb"], ["QTt"])
            yield
            p4 = P[4][:, :].rearrange("p (h t c) -> p h t c", h=HG, t=2)
            p5 = P[5][:, 0:HG * 128].rearrange("p (h c) -> p h c", h=HG)
            for m in range(5):
                for h in range(HG):
                    MM(P[4][:, h * 256:h * 256 + 128], Pb[:, h, :], Qb[:, h, :], True, True, ["Pb", "QTq"], ["P4"])
                    MM(P[4][:, h * 256 + 128:(h + 1) * 256], Pb[:, h, :], Tb[:, h, :], True, True, ["Pb", "QTt"], ["P4"])
                for h in range(HG):
                    MM(P[5][:, h * 128:(h + 1) * 128], Qb[:, h, :], Pb[:, h, :], True, True, ["Pb", "QTq"], ["P5"])
                TT("dve", Tb[:], Tb[:], p4[:, :, 1, :], ALU.add, ["QTt", "P4"], ["QTt"])
                for h in range(HG):
                    CP("dve", Qb[:, h, :], P[4][:, h * 256:h * 256 + 128], ["P4"], ["QTq"])
                CP("dve", Pb[:].rearrange("p h c -> p (h c)"), P[5][:, 0:HG * 128], ["P5"], ["Pb"])
                yield
            for h in range(HG):
                MM(P[5][:, h * 128:(h + 1) * 128], Pb[:, h, :], Tb[:, h, :], True, True, ["Pb", "QTt"], ["P5"])
            TT("dve", Tb[:], Tb[:], p5, ALU.add, ["QTt", "P5"], ["QTt"])
            for h in range(HG):
                MM(P[3][:, h * 128:(h + 1) * 128], Tb[:, h, :], vbb[:, h, :], True, True, ["QTt", "vbb"], ["P3"])
                MM(P[5][:, h * 128:(h + 1) * 128], kbgb[:, h, :], Tb[:, h, :], True, True, ["QTt", "kbgb"], ["P5"])
            CP("dve", usb[:], P[3][:, 0:HG * 128], ["P3"], [ku])
            CP("dve", wTb[:].rearrange("p h c -> p (h c)"), P[5][:, 0:HG * 128], ["P5"], [kw])

        def dn_scan(g, i, d, lat, pb):
            tc0 = i * 128
            e0 = g * HG
            exs, ATb, kdb, wTb, usb = exs2[pb], ATb2[pb], kdb2[pb], wTb2[pb], usb2[pb]
            kx, ka, kk_, kw, ku = "exs%d" % pb, "ATb%d" % pb, "kdb%d" % pb, "wTb%d" % pb, "usb%d" % pb
            for c in ([0, 1] if d == 0 else [1, 0]):
                pr = slice(c * 64, (c + 1) * 64)
                for h in range(HG):
                    MM(P[6][pr, h * 128:(h + 1) * 128], wTb[:, h, pr], Sb[d][h][:], True, True, [kw, "S%d%d" % (d, h)], ["P6"])
                if lat:
                    for h in range(HG):
                        MM(P[6][pr, 256 + h * 128:256 + (h + 1) * 128], qT[:, h, tc0 + c * 64:tc0 + (c + 1) * 64], Sb[d][h][:], True, True,
                           ["qkv", "S%d%d" % (d, h)], ["P6"])
                TT("dve", vn[pr].rearrange("p h c -> p (h c)"), usb[pr, :], P[6][pr, 0:HG * 128], ALU.subtract, [ku, "P6"], ["vn"])
                yield
                if lat:
                    for h in range(HG):
                        TS("dve", osb[pr, h * 128:(h + 1) * 128], P[6][pr, 256 + h * 128:256 + (h + 1) * 128], exs[pr, e0 + h:e0 + h + 1],
                           ALU.mult, ["P6", kx], ["osb"])
                    for h in range(HG):
                        MM(P[0][pr, h * 128:(h + 1) * 128], ATb[pr, h, pr], vn[pr, h, :], True, True, [ka, "vn"], ["P0"])
                for h in range(HG):
                    MM(P[0][:, 256 + h * 128:256 + (h + 1) * 128], kdb[pr, h, :], vn[pr, h, :], True, True, [kk_, "vn"], ["P0"])
                if lat:
                    oview = ostore[pr, i, :, :].rearrange("p h c -> p (h c)")
                    if d == 0:
                        TT("dve", oview, osb[pr, :], P[0][pr, 0:256], ALU.add, ["osb", "P0"], ["ostore"])
                    else:
                        TT("dve", otmp[pr, :], osb[pr, :], P[0][pr, 0:256], ALU.add, ["osb", "P0"], ["otmp"])
                        TT("pool", oview, oview, otmp[pr, :], ALU.add, ["otmp", "ostore"], ["ostore"])
                for h in range(HG):
                    STT(Sb[d][h][:], Sb[d][h][:], exs[:, 4 + 4 * c + e0 + h:5 + 4 * c + e0 + h], P[0][:, 256 + h * 128:256 + (h + 1) * 128],
                        ALU.mult, ALU.add, ["S%d%d" % (d, h), kx, "P0"], ["S%d%d" % (d, h)])
                yield


        def dn_run(tiles):
            def drain(gen):
                for _ in gen:
                    pass
            prev = None
            for n, (g, i, d, lat) in enumerate(tiles):
                pg = dn_prep(g, i, d, lat, n % 2)
                if prev is None:
                    drain(pg)
                else:
                    sg = dn_scan(*prev)
                    alive_p, alive_s = True, True
                    while alive_p or alive_s:
                        if alive_p:
                            alive_p = next(pg, "end") != "end"
                        if alive_s:
                            alive_s = next(sg, "end") != "end"
                prev = (g, i, d, lat, n % 2)
            drain(dn_scan(*prev))

        tmpd = sb(stA, "tmpd", [128, 512], F32)

        def conformer():
            for cc in range(4):
                wa, wak = load_w(2064 + cc * 128)
                wgt, wgk = load_w(2576 + cc * 128)
                for t in range(4):
                    proj_tile(wa, wak, xnT, "xnT", t * 512, 512, P[1], "P1")
                    proj_tile(wgt, wgk, xnT, "xnT", t * 512, 512, P[2], "P2")
                    ACT(tmpa[:], P[2][:, :], AF.Sigmoid, ["P2"], ["tmpa"])
                    TT("dve", ybuf[:, 960 + t * 512:960 + (t + 1) * 512], P[1][:, :], tmpa[:], ALU.mult, ["P1", "tmpa"], ["ybuf"])
                for jt in range(31):
                    TS("dve", dgc[:, jt, :], identb[:], cfw[:, cc, jt:jt + 1], ALU.mult, ["identb", "consts"], ["dgc"])
                for t in range(4):
                    for jt in range(31):
                        MM(P[1][:, :], dgc[:, jt, :], ybuf[:, t * 512 + jt * 64:t * 512 + jt * 64 + 512], jt == 0, jt == 30,
                           ["dgc", "ybuf"], ["P1"])
                    ACT(cfT[:, cc, t * 512:(t + 1) * 512], P[1][:, :], AF.Identity, ["P1", "consts"], ["cfT"], bias=cfv[:, 0, cc:cc + 1])
            for t in range(4):
                sl = slice(t * 512, (t + 1) * 512)
                for cc in range(4):
                    MM(P[5][:, :], onesb[:], cfT[:, cc, sl], cc == 0, cc == 3, ["onesb", "cfT"], ["P5"])
                for cc in range(4):
                    TT("dve", tmpc[:], cfT[:, cc, sl], cfT[:, cc, sl], ALU.mult, ["cfT"], ["tmpc"])
                    MM(P[6][:, :], onesb[:], tmpc[:], cc == 0, cc == 3, ["onesb", "tmpc"], ["P6"])
                TS("dve", tmpa[:], P[5][:, :], 1.0 / 512, ALU.mult, ["P5"], ["tmpa"])
                TT("dve", tmpb[:], tmpa[:], tmpa[:], ALU.mult, ["tmpa"], ["tmpb"])
                STT(tmpb[:], P[6][:, :], 1.0 / 512, tmpb[:], ALU.mult, ALU.subtract, ["P6", "tmpb"], ["tmpb"])
                ACT(tmpb[:], tmpb[:], AF.Sqrt, ["tmpb"], ["tmpb"], bias=EPS)
                RCP(tmpb[:], tmpb[:], ["tmpb"], ["tmpb"])
                for cc in range(4):
                    TT("dve", tmpd[:], cfT[:, cc, sl], tmpa[:], ALU.subtract, ["cfT", "tmpa"], ["tmpd"])
                    TT("dve", tmpd[:], tmpd[:], tmpb[:], ALU.mult, ["tmpd", "tmpb"], ["tmpd"])
                    ACT(cfT[:, cc, sl], tmpd[:], AF.Silu, ["tmpd", "consts"], ["cfT"], bias=cfv[:, 2, cc:cc + 1], scale=cfv[:, 1, cc:cc + 1])

        def dn_finish(g):
            for i in range(16):
                for h in range(HG):
                    ACT(junk[:, 0:128], ostore[:, i, h, :], AF.Square, ["ostore"], ["junk", "ms_a"], accum=ms[:, h:h + 1])
                ACT(ms[:, 2:4], ms[:, 0:2], AF.Sqrt, ["ms_a"], ["ms_b"], bias=EPS, scale=1.0 / 128)
                RCP(ms[:, 4:6], ms[:, 2:4], ["ms_b"], ["ms_c"])
                for h in range(HG):
                    TS("dve", ont[:, h, :], ostore[:, i, h, :], ms[:, 4 + h:5 + h], ALU.mult, ["ostore", "ms_c"], ["ont"])
                for h in range(HG):
                    TR(ps_tr[:, 768 + h * 128:768 + (h + 1) * 128], ont[:, h, :], identb[:], ["ont", "identb"], ["ps_tr"])
                for h in range(HG):
                    STT(dnT[:, g * HG + h, i * 128:(i + 1) * 128], ps_tr[:, 768 + h * 128:768 + (h + 1) * 128], normg[:, 0:1],
                        zsT[:, h, i * 128:(i + 1) * 128], ALU.mult, ALU.mult, ["ps_tr", "consts", "zsT"], ["dnT"])

        for i in range(2):
            DMA("sp", l_x, xin[:], ctx_d[s, i * 128:(i + 1) * 128, :], [], ["xin"])
            norm_mod_T(xin[:], "xin", 2, G1, 0, xnTc, i * 128, "xnTc")
        for i in range(16):
            DMA("sp", l_x, xin[:], x_d[s, i * 128:(i + 1) * 128, :], [], ["xin"])
            norm_mod_T(xin[:], "xin", s, G1, 0, xnT, i * 128, "xnT")
        dump("xnT", xnT[:], [128, 8, 2048], BF16, "xnT")
        if stage == 0.5:
            S.barrier(); stA.close(); continue
        ba_all(256, xnTc, "xnTc", 16)
        ba_all(2048, xnT, "xnT", 0)
        dump("gall", gall[:], [128, 288], F32, "gall")
        dump("ball", ball[:], [128, 288], F32, "ball")
        if stage == 0.7:
            S.barrier(); stA.close(); continue
        if stage >= 2:
            conformer()
            dump("cfT", cfT[:], [128, 4, 2048], BF16, "cfT")
        for g in range(4 // HG):
            for d in range(2):
                for h in range(HG):
                    MSET("pool", Sb[d][h][:], 0.0, ["S%d%d" % (d, h)])
            for h in range(HG):
                conv_chunk(4 + g * HG + h, 256, "k", kT[:, h, 0:256], xnTc, "xnTc")
                conv_chunk(8 + g * HG + h, 256, "v", cvT[:, h, 0:256], xnTc, "xnTc")
            dn_run([(g, 0, 0, False), (g, 1, 0, False), (g, 1, 1, False), (g, 0, 1, False)])
            if g == 0:
                dump("kTc", kT[:, :, 0:256], [128, HG, 256], BF16, "qkv")
                dump("Sctx", Sb[0][0][:], [128, 128], BF16, "S00")
            for h in range(HG):
                hh = g * HG + h
                wz, wzk = load_w(1536 + hh * 128)
                for t in range(4):
                    proj_tile(wz, wzk, xnT, "xnT", t * 512, 512, P[1], "P1")
                    ACT(zsT[:, h, t * 512:(t + 1) * 512], P[1][:, :], AF.Silu, ["P1"], ["zsT"])
                conv_chunk(hh, 2048, "q", qT[:, h, :], xnT, "xnT")
                conv_chunk(4 + hh, 2048, "k", kT[:, h, :], xnT, "xnT")
                conv_chunk(8 + hh, 2048, "v", cvT[:, h, :], xnT, "xnT")
            if g == 0:
                dump("qT", qT[:], [128, HG, 2048], BF16, "qkv")
                dump("kT", kT[:], [128, HG, 2048], BF16, "qkv")
                dump("cvT", cvT[:], [128, HG, 2048], BF16, "qkv")
            dn_run([(g, i, 0, True) for i in range(16)] + [(g, i, 1, True) for i in range(15, -1, -1)])
            if g == 0:
                dump("ostore", ostore[:], [128, 16, HG, 128], F32, "ostore")
            dn_finish(g)
        dump("dnT", dnT[:], [128, 4, 2048], BF16, "dnT")
        if stage <= 2:
            S.barrier()
            stA.close()
            continue
        wo_h = [qT[:].rearrange("p a (b c) -> p (a b) c", c=512), kT[:].rearrange("p a (b c) -> p (a b) c", c=512)]
        gt1bc = sb(stA, "gt1bc", [128, 1024], F32)
        rbt = tmpd[:].rearrange("p (a c) -> p a c", c=128)
        xhalf = [tmpa, tmpb]
        hTt1 = sb(stA, "hTt", [128, 8, 128], BF16)
        hTt = [hTt1, hTt1]
        l_x1 = S.lane("x1st%d" % s)
        l_h = [S.lane("hst%d_%d" % (s, i)) for i in range(2)]
        for c8 in range(8):
            b = wctr[0] % 2
            wctr[0] += 1
            DMA("sp", l_w[b], wst[b][:], wout_d.rearrange("(k p) c -> p k c", p=128)[:, :, c8 * 128:(c8 + 1) * 128], [], ["wst%d" % b])
            CP("pool", wo_h[c8 // 4][:, :, (c8 % 4) * 128:(c8 % 4 + 1) * 128], wst[b][:], ["wst%d" % b], ["qkv"])
        row_bcast(gt1bc, lambda k: modS[:, 16 + k, s:s + 1], rbt, ["modS", "tmpd"], "gt1bc", "tmpd")
        hTv = hT_d.rearrange("(k p) t -> p k t", p=128)
        for i in range(16):
            ts_ = slice(i * 128, (i + 1) * 128)
            DMA("sp", l_x, xin[:], x_d[s, ts_, :], [], ["xin"])
            for half in range(2):
                for k in range(8):
                    src = dnT[:, k, ts_] if k < 4 else cfT[:, k - 4, ts_]
                    MM(P[1 + half][:, :], src, wo_h[half][:, k, :], k == 0, k == 7,
                       ["dnT", "cfT", "qkv"], ["P%d" % (1 + half)])
                hk = "tmpa" if half == 0 else "tmpb"
                TT("dve", xhalf[half][:], P[1 + half][:, :], gt1bc[:, half * 512:(half + 1) * 512], ALU.mult,
                   ["P%d" % (1 + half), "gt1bc"], [hk])
                TT("pool", xin[:, half * 512:(half + 1) * 512], xin[:, half * 512:(half + 1) * 512], xhalf[half][:], ALU.add,
                   [hk, "xin"], ["xin"])
            DMA("sp", l_x1, x1_d[ts_, :], xin[:], ["xin"], ["x1_d"])
            b = i % 2
            norm_mod_T(xin[:], "xin", s, G2, 24, hTt[b], 0, "hTt")
            DMA("sp", l_h[b], hTv[:, :, ts_], hTt[b][:], ["hTt"], ["hT_d"])
        S.barrier()
        stA.close()

        stM = ExitStack()
        hT = sb(stM, "hT", [128, 8, 2048], BF16)
        acc = sb(stM, "acc", [128, 16, 1024], F32)
        Wr = sb(stM, "Wr", [128, 16, 64], F32)
        l_m = S.lane("moe_in%d" % s)
        DMA("sp", l_m, hT[:], hTv, ["hT_d"], ["hT"])
        with ExitStack() as stR:
            scores = sb(stR, "scores", [128, 16, 64], F32)
            sel = sb(stR, "sel", [128, 16, 64], F32)
            sel2 = sb(stR, "sel2", [128, 16, 64], F32)
            m1 = sb(stR, "m1", [128, 128], F32)
            m2 = sb(stR, "m2", [128, 128], F32)
            top8 = sb(stR, "top8", [128, 16, 8], F32)
            keepg = sb(stR, "keepg", [128, 128], F32)
            nrm = sb(stR, "nrm", [128, 16], F32)
            for i in range(16):
                for k in range(8):
                    MM(P[0][:, (i % 8) * 64:(i % 8 + 1) * 64], hT[:, k, i * 128:(i + 1) * 128], rwb[:, k, :], k == 0, k == 7, ["hT", "rwb"], ["P0"])
                if i % 8 == 7:
                    ACT(scores[:, i - 7:i + 1, :].rearrange("p a e -> p (a e)"), P[0][:, :], AF.Sigmoid, ["P0"], ["scores"])
            TT("dve", sel[:], scores[:], rbias[:, :].unsqueeze(1).to_broadcast([128, 16, 64]), ALU.add, ["scores", "consts"], ["sel"])
            selg = sel[:].rearrange("p a (g e) -> p (a g) e", e=8)
            sel2g = sel2[:].rearrange("p a (g e) -> p (a g) e", e=8)
            S.op("dve", lambda e: e.tensor_reduce(out=m1[:], in_=selg, axis=AX.X, op=ALU.max), ["sel"], ["m1"])
            TT("dve", sel2g, selg, m1[:, :].unsqueeze(2).to_broadcast([128, 128, 8]), ALU.is_equal, ["sel", "m1"], ["sel2"])
            STT(sel2[:], sel2[:], -1e9, sel[:], ALU.mult, ALU.add, ["sel2", "sel"], ["sel2"])
            S.op("dve", lambda e: e.tensor_reduce(out=m2[:], in_=sel2g, axis=AX.X, op=ALU.max), ["sel2"], ["m2"])
            TT("dve", m1[:], m1[:], m2[:], ALU.add, ["m1", "m2"], ["m1"])
            for i in range(16):
                S.op("dve", lambda e: e.max(out=top8[:, i, :], in_=m1[:, i * 8:(i + 1) * 8]), ["m1"], ["top8"])
            TT("dve", keepg[:].rearrange("p (a g) -> p a g", g=8), m1[:].rearrange("p (a g) -> p a g", g=8),
               top8[:, :, 3:4].to_broadcast([128, 16, 8]), ALU.is_ge, ["m1", "top8"], ["keepg"])
            TS("dve", sel2[:], sel[:], 10.0, ALU.add, ["sel"], ["sel2"])
            TT("dve", sel2g, sel2g, keepg[:, :].unsqueeze(2).to_broadcast([128, 128, 8]), ALU.mult, ["sel2", "keepg"], ["sel2"])
            TS("dve", sel2[:], sel2[:], -10.0, ALU.add, ["sel2"], ["sel2"])
            for i in range(16):
                S.op("dve", lambda e: e.max(out=top8[:, i, :], in_=sel2[:, i, :]), ["sel2"], ["top8"])
            TT("dve", sel[:], sel2[:], top8[:, :, 5:6].to_broadcast([128, 16, 64]), ALU.is_ge, ["sel2", "top8"], ["sel"])
            TT("dve", sel[:], sel[:], scores[:], ALU.mult, ["sel", "scores"], ["sel"])
            S.op("dve", lambda e: e.tensor_reduce(out=nrm[:], in_=sel[:], axis=AX.X, op=ALU.add), ["sel"], ["nrm"])
            RCP(nrm[:], nrm[:], ["nrm"], ["nrm"])
            STT(Wr[:], sel[:], 2.5, nrm[:, :].unsqueeze(2).to_broadcast([128, 16, 64]), ALU.mult, ALU.mult, ["sel", "nrm"], ["Wr"])
            dump("Wr", Wr[:], [128, 16, 64], F32, "Wr")
            S.barrier()
        wgs = sb(stM, "wgs", [128, 8, 512], F32)
        wds = sb(stM, "wds", [128, 2, 1024], F32)
        wgb = [sb(stM, "wgb%d" % i, [128, 8, 512], BF16) for i in range(2)]
        wdb = [sb(stM, "wdb%d" % i, [128, 2, 1024], BF16) for i in range(2)]
        sgb = sb(stM, "sgb", [128, 512], BF16)
        actb = sb(stM, "actb", [128, 2, 512], BF16)
        l_e = [S.lane("exg%d" % s), S.lane("exd%d" % s)]
        n_exp = 65 if stage >= 4 else 2
        def exid(e_):
            return e_ if stage >= 4 else (0 if e_ == 0 else 64)

        def load_expert(e_):
            ex = exid(e_)
            b = e_ % 2
            S.dma("sp", l_e[0], [
                lambda e: e.dma_start(out=wgs[:, :, 0:256], in_=wg_d[ex].rearrange("(k p) c -> p k c", p=128)),
                lambda e: e.dma_start(out=wgs[:, :, 256:512], in_=wu_d[ex].rearrange("(k p) c -> p k c", p=128)),
            ], [], ["wgs"])
            DMA("sp", l_e[1], wds[:], wd_d[ex].rearrange("(k p) c -> p k c", p=128), [], ["wds"])
            CP("pool", wgb[b][:], wgs[:], ["wgs"], ["wgb%d" % b])
            CP("pool", wdb[b][:], wds[:], ["wds"], ["wdb%d" % b])

        def gate_up(e_, t, ab):
            b = e_ % 2
            tsl = slice(t * 512, (t + 1) * 512)
            for c in range(2):
                for k in range(8):
                    MM(P[1 + c][:, :], wgb[b][:, k, c * 128:(c + 1) * 128], hT[:, k, tsl], k == 0, k == 7, ["wgb%d" % b, "hT"], ["P%d" % (1 + c)])
                for k in range(8):
                    MM(P[3 + c][:, :], wgb[b][:, k, 256 + c * 128:256 + (c + 1) * 128], hT[:, k, tsl], k == 0, k == 7,
                       ["wgb%d" % b, "hT"], ["P%d" % (3 + c)])
            for c in range(2):
                ACT(sgb[:], P[1 + c][:, :], AF.Silu, ["P%d" % (1 + c)], ["sgb"])
                TT("dve", actb2[ab][:, c, :], P[3 + c][:, :], sgb[:], ALU.mult, ["P%d" % (3 + c), "sgb"], ["actb%d" % ab])

        def down(e_, t, ab):
            ex = exid(e_)
            b = e_ % 2
            for j in range(4):
                i = t * 4 + j
                for half in range(2):
                    pb = P[5 + half]
                    pk = "P%d" % (5 + half)
                    for c in range(2):
                        MM(pb[:, :], actb2[ab][:, c, j * 128:(j + 1) * 128], wdb[b][:, c, half * 512:(half + 1) * 512], c == 0, c == 1,
                           ["actb%d" % ab, "wdb%d" % b], [pk])
                    av = acc[:, i, half * 512:(half + 1) * 512]
                    wsc = Wr[:, i, ex:ex + 1] if ex < 64 else 1.0
                    if e_ == 0:
                        TS("dve", av, pb[:, :], wsc, ALU.mult, [pk, "Wr"], ["acc"])
                    else:
                        STT(av, pb[:, :], wsc, av, ALU.mult, ALU.add, [pk, "Wr", "acc"], ["acc"])

        actb2 = [actb, sb(stM, "actb1", [128, 2, 512], BF16)]
        pairs = [(e_, t) for e_ in range(n_exp) for t in range(4)]
        load_expert(0)
        prevp = None
        for n, (e_, t) in enumerate(pairs):
            if t == 1 and e_ + 1 < n_exp:
                load_expert(e_ + 1)
            gate_up(e_, t, n % 2)
            if prevp is not None:
                down(*prevp)
            prevp = (e_, t, n % 2)
        down(*prevp)
        gt2bc = sb(stM, "gt2bc", [128, 1024], F32)
        gfbc = sb(stM, "gfbc", [128, 1024], F32)
        rbt2 = sb(stM, "rbt2", [128, 4, 128], F32)
        xin2 = sb(stM, "xin2", [128, 1024], F32)
        ot = [sb(stM, "ot%d" % i, [128, 1024], F32) for i in range(2)]
        junk2 = sb(stM, "junk2", [128, 1024], BF16)
        st2 = sb(stM, "st2", [128, 4], F32)
        l_x2 = S.lane("x1ld%d" % s)
        l_o = [S.lane("out%d_%d" % (s, i)) for i in range(2)]
        row_bcast(gt2bc, lambda k: modS[:, 40 + k, s:s + 1], rbt2, ["modS"], "gt2bc")
        row_bcast(gfbc, lambda k: gvec[:, 2, k:k + 1], rbt2, ["consts"], "gfbc")
        for i in range(16):
            ts_ = slice(i * 128, (i + 1) * 128)
            b = i % 2
            DMA("sp", l_x2, xin2[:], x1_d[ts_, :], ["x1_d"], ["xin2"])
            TT("dve", acc[:, i, :], acc[:, i, :], gt2bc[:], ALU.mult, ["acc", "gt2bc"], ["acc"])
            TT("pool", acc[:, i, :], acc[:, i, :], xin2[:], ALU.add, ["acc", "xin2"], ["acc"])
            ACT(junk2[:], acc[:, i, :], AF.Square, ["acc"], ["junk2", "st2a"], accum=st2[:, 0:1])
            ACT(st2[:, 1:2], st2[:, 0:1], AF.Sqrt, ["st2a"], ["st2b"], bias=EPS, scale=1.0 / 1024)
            RCP(st2[:, 2:3], st2[:, 1:2], ["st2b"], ["st2c"])
            STT(ot[b][:], acc[:, i, :], st2[:, 2:3], gfbc[:], ALU.mult, ALU.mult, ["acc", "st2c", "gfbc"], ["ot%d" % b])
            DMA("sp", l_o[b], out_d[s, ts_, :], ot[b][:], ["ot%d" % b], [])
        S.barrier()
        stM.close()
    S.barrier()
    st0.close()
    return nc, dump_d


def _masks():
    idx = np.arange(128)
    k = idx[:, None]; i = idx[None, :]
    sc = (k // 64) == (i // 64)
    m = np.zeros((128, 12, 128), np.float32)
    m[:, 0] = sc & (k <= i)
    m[:, 1] = sc & (k >= i)
    m[:, 2] = sc & (k > i)
    m[:, 3] = sc & (k < i)
    m[:, 4] = np.where(sc & (k > i), 0.0, NEG)
    m[:, 5] = np.where(sc & (k < i), 0.0, NEG)
    m[:, 6] = np.where(sc & (i >= k), 0.0, NEG)
    m[:, 7] = np.where(sc & (i <= k), 0.0, NEG)
    m[:, 8] = (k < 64) & (i >= 0)
    m[:, 9] = (k >= 64) & (i >= 0)
    m[:, 10] = (k == i)
    m[:, 11] = 1.0
    negrep = np.stack([np.tile(m[:, 4 + q], (1, HG)) for q in range(4)], axis=1).astype(np.float32)
    return m, np.ascontiguousarray(negrep)


def _prep_inputs(inp, core):
    f = lambda a: np.ascontiguousarray(a, dtype=np.float32)
    b0 = 2 * core
    cT = np.zeros((128, 8, 4), np.float32)
    cT[:, :, 0] = inp["c"][b0].reshape(8, 128).T
    cT[:, :, 1] = inp["c"][b0 + 1].reshape(8, 128).T
    cT[:, :, 2] = inp["c_ctx"].reshape(8, 128).T
    gvec = np.stack([inp["g_mix"][0], inp["g_ffn"][0], inp["g_final"]]).reshape(3, 8, 128).transpose(2, 0, 1)
    r0 = np.zeros(16, np.float32); r1 = np.zeros(16, np.float32)
    for d in range(2):
        for h in range(4):
            r0[d * 8 + 4 + h] = inp["dn_dt_bias"][0, d, h]
            r1[d * 8 + 4 + h] = inp["dn_a_log"][0, d, h]
    bac = np.broadcast_to(np.stack([np.tile(r0, 16), np.tile(r1, 16)])[None], (128, 2, 256))
    masks, negrep = _masks()
    cfv = np.stack([inp["cf_dw_b"][0], inp["cf_ln_g"][0], inp["cf_ln_b"][0]]).reshape(3, 4, 128).transpose(2, 0, 1)
    return {
        "x": f(inp["x"][b0:b0 + 2]), "ctx": f(inp["ctx"][b0:b0 + 2]), "cT": cT,
        "w_mod": f(inp["w_mod"][0]), "b_modT": f(inp["b_mod"][0].reshape(48, 128).T),
        "gvec": f(gvec), "w_in": f(inp["w_in"][0]), "w_out": f(inp["w_out"][0]),
        "convw": f(inp["dn_conv_w"][0].reshape(7, 12, 128).transpose(2, 1, 0)),
        "bac": f(bac), "normg": f(inp["dn_norm_g"][0].reshape(128, 1)),
        "cfw": f(inp["cf_dw_w"][0].reshape(31, 4, 128).transpose(2, 1, 0)), "cfv": f(cfv),
        "router_w": f(inp["router_w"][0]), "rbias": f(np.broadcast_to(inp["router_bias"][0][None], (128, 64))),
        "wg": inp["_wg"], "wu": inp["_wu"], "wd": inp["_wd"],
        "masks": masks, "negrep": negrep,
    }


def kernel(**inputs):
    inp = {k: np.asarray(v) for k, v in inputs.items()}
    inp["_wg"] = np.ascontiguousarray(np.concatenate([inp["exp_w_gate"][0], inp["sh_w_gate"]], axis=0), dtype=np.float32)
    inp["_wu"] = np.ascontiguousarray(np.concatenate([inp["exp_w_up"][0], inp["sh_w_up"]], axis=0), dtype=np.float32)
    inp["_wd"] = np.ascontiguousarray(np.concatenate([inp["exp_w_down"][0], inp["sh_w_down"]], axis=0), dtype=np.float32)
    nc, _ = build_nc()
    in_maps = [_prep_inputs(inp, c) for c in range(8)]
    res = run_bass_kernel_spmd(nc, in_maps, core_ids=list(range(8)))
    out = np.concatenate([r["out"] for r in res.results], axis=0)
    return out.astype(np.float32)
```
